# Optimizing a Trainium2 kernel written in Bass

```python
import math
import jax
import jax.numpy as jnp
from jax import lax
import numpy as np

D_MODEL = 1024
BATCH = 16
SEQ = 2048
DEPTH = 2

GMLP_WIDTH = D_MODEL // 4
GMLP_GROUPS = 4
GMLP_CHUNK = 128
POOL_WIDTH = D_MODEL // 4
POOL_WINDOWS = (2, 4, 8, 16)
POOL_GROUP = POOL_WIDTH // len(POOL_WINDOWS)
HEAD_DIM = 64
ATTN_HEADS = D_MODEL // 128
ATTN_WIDTH = ATTN_HEADS * HEAD_DIM
DILATED_CONFIGS = ((128, 1), (512, 4), (2048, 16))
ATTN_BLOCK = 128
ALIBI_MAX_BIAS = 8.0
MASK_VALUE = -1e30
CONV_WIDTH = D_MODEL // 4
CONV_KERNEL = 31
N_BRANCHES = 4
_A_END = 2 * GMLP_WIDTH
_B_END = _A_END + POOL_WIDTH
_Q_END = _B_END + ATTN_WIDTH
_K_END = _Q_END + ATTN_WIDTH
_V_END = _K_END + ATTN_WIDTH
IN_WIDTH = _V_END + 2 * CONV_WIDTH
IN_SPLITS = (_A_END, _B_END, _Q_END, _K_END, _V_END)
D_FF_DENSE = ((8 * D_MODEL // 3 + 127) // 128) * 128
N_EXPERTS = 8
TOP_K = 2
D_FF_EXPERT = 7 * D_MODEL // 2
N_DENSE = (DEPTH + 1) // 2
N_MOE = DEPTH // 2
RMS_EPS = 1e-6
LN_EPS = 1e-5

kernel_name = 'hybrid_gated_mixers_moe'


def rmsnorm(x, g):
    x32 = x.astype(jnp.float32)
    y = x32 * lax.rsqrt(jnp.mean(x32 * x32, axis=-1, keepdims=True) + RMS_EPS)
    return (y * g.astype(jnp.float32)).astype(x.dtype)


def layernorm(x, g, b):
    x32 = x.astype(jnp.float32)
    mu = jnp.mean(x32, axis=-1, keepdims=True)
    var = jnp.mean(jnp.square(x32 - mu), axis=-1, keepdims=True)
    y = (x32 - mu) * lax.rsqrt(var + LN_EPS)
    return (y * g.astype(jnp.float32) + b.astype(jnp.float32)).astype(x.dtype)


def swiglu(x, w1, w3, w2):
    return (jax.nn.silu(x @ w1) * (x @ w3)) @ w2


def alibi_slopes(n_heads):
    return 2.0 ** (-ALIBI_MAX_BIAS * jnp.arange(1, n_heads + 1, dtype=jnp.float32) / n_heads)


def gmlp_spatial_gating(z, ln_g, ln_b, w_s, b_s):
    b, s, _ = z.shape
    u, v = jnp.split(z, 2, axis=-1)
    v = layernorm(v, ln_g, ln_b)
    v = v.reshape(b, s // GMLP_CHUNK, GMLP_CHUNK, GMLP_GROUPS, GMLP_WIDTH // GMLP_GROUPS)
    causal = jnp.tril(jnp.ones((GMLP_CHUNK, GMLP_CHUNK), dtype=bool))
    w = jnp.where(causal, w_s, 0.0).astype(v.dtype)
    mixed = jnp.einsum('gts,bcsgd->bctgd', w, v) + b_s.T.astype(v.dtype)[None, None, :, :, None]
    return u * mixed.reshape(b, s, GMLP_WIDTH)


def multiscale_pool(xp, w_pool, b_pool, scale):
    b, s, _ = xp.shape
    x32 = xp.astype(jnp.float32).reshape(b, s, len(POOL_WINDOWS), POOL_GROUP)
    csum = jnp.cumsum(x32, axis=1)
    pos1 = jnp.arange(1, s + 1, dtype=jnp.float32)
    outs = []
    for g, w in enumerate(POOL_WINDOWS):
        c = csum[:, :, g]
        c_lag = jnp.pad(c, ((0, 0), (w, 0), (0, 0)))[:, :s]
        count = jnp.minimum(pos1, float(w))[None, :, None]
        outs.append((c - c_lag) / count - x32[:, :, g])
    pooled = jnp.stack(outs, axis=2)
    y = jnp.einsum('bsgc,gcd->bsgd', pooled, w_pool.astype(jnp.float32)) + b_pool.astype(jnp.float32)
    return (y.reshape(b, s, POOL_WIDTH) * scale.astype(jnp.float32)).astype(xp.dtype)


def dilated_window_attention(q, k, v, window, dilation, slopes):
    b, s, h, dh = q.shape
    steps = window // dilation
    sub_len = s // dilation
    n_blk = -(-sub_len // ATTN_BLOCK)
    sub_pad = n_blk * ATTN_BLOCK

    def to_blocks(t):
        t = t.reshape(b, sub_len, dilation, h, dh).transpose(0, 2, 1, 3, 4)
        t = jnp.pad(t, ((0, 0), (0, 0), (0, sub_pad - sub_len), (0, 0), (0, 0)))
        return t.reshape(b, dilation, n_blk, ATTN_BLOCK, h, dh)

    def with_prev(t):
        prev = jnp.pad(t[:, :, :-1], ((0, 0), (0, 0), (1, 0), (0, 0), (0, 0), (0, 0)))
        return jnp.concatenate([prev, t], axis=3)

    qb = to_blocks(q)
    kk = with_prev(to_blocks(k))
    vv = with_prev(to_blocks(v))
    scores = jnp.einsum('brnqhd,brnkhd->brnqhk', qb, kk,
                        preferred_element_type=jnp.float32) / math.sqrt(dh)
    qi = jnp.arange(ATTN_BLOCK)[:, None]
    kj = jnp.arange(2 * ATTN_BLOCK)[None, :]
    dist = qi + ATTN_BLOCK - kj
    blk = jnp.arange(n_blk)[:, None, None]
    key_pos = blk * ATTN_BLOCK + kj[None] - ATTN_BLOCK
    valid = (dist >= 0)[None] & (dist <= steps)[None] & (key_pos >= 0)
    alibi = -slopes[None, :, None] * (dist * dilation).astype(jnp.float32)[:, None, :]
    scores = jnp.where(valid[:, :, None, :], scores + alibi, MASK_VALUE)
    m = jnp.max(scores, axis=-1, keepdims=True)
    p = jnp.exp(scores - m)
    den = jnp.sum(p, axis=-1)
    o = jnp.einsum('brnqhk,brnkhd->brnqhd', p, vv.astype(jnp.float32)) / den[..., None]
    lse = m[..., 0] + jnp.log(den)

    def from_blocks(t):
        t = t.reshape((b, dilation, sub_pad) + t.shape[4:])[:, :, :sub_len]
        t = jnp.moveaxis(t, 1, 2)
        return t.reshape((b, s) + t.shape[3:])

    return from_blocks(o), from_blocks(lse)


def dilated_mixture_attention(q, k, v, slopes):
    outs, lses = [], []
    for window, dilation in DILATED_CONFIGS:
        o, lse = dilated_window_attention(q, k, v, window, dilation, slopes)
        outs.append(o)
        lses.append(lse)
    alpha = jax.nn.softmax(jnp.stack(lses, axis=0), axis=0)
    o = jnp.sum(alpha[..., None] * jnp.stack(outs, axis=0), axis=0)
    return o.astype(q.dtype)


def conformer_conv(z, w_dw, b_dw, ln_g, ln_b):
    a, g = jnp.split(z, 2, axis=-1)
    y = a * jax.nn.sigmoid(g)
    y = lax.conv_general_dilated(
        y, w_dw[:, None, :].astype(y.dtype), window_strides=(1,),
        padding=((CONV_KERNEL - 1, 0),), dimension_numbers=('NWC', 'WIO', 'NWC'),
        feature_group_count=CONV_WIDTH) + b_dw.astype(y.dtype)
    y = layernorm(y, ln_g, ln_b)
    return jax.nn.silu(y)


def moe_swiglu(h, w_router, w1, w3, w2):
    b, s, d = h.shape
    t = h.reshape(b * s, d)
    logits = jnp.dot(t, w_router, preferred_element_type=jnp.float32)
    top_val, top_idx = lax.top_k(logits, TOP_K)
    top_w = jax.nn.softmax(top_val, axis=-1)
    combine = jnp.sum(jax.nn.one_hot(top_idx, N_EXPERTS, dtype=jnp.float32) * top_w[..., None], axis=1)
    out = jnp.zeros((b * s, d), jnp.float32)
    for e in range(N_EXPERTS):
        out = out + combine[:, e:e + 1] * swiglu(t, w1[e], w3[e], w2[e]).astype(jnp.float32)
    return out.reshape(b, s, d).astype(h.dtype)


def setup_inputs(seed: int = 0) -> dict:
    key = jax.random.key(seed)
    ks = iter(jax.random.split(key, 40))

    def nrm(shape, scale):
        return jax.random.normal(next(ks), shape, jnp.float32) * scale

    def gain(shape):
        return 1.0 + nrm(shape, 0.1)

    L = DEPTH
    return {
        'x': nrm((BATCH, SEQ, D_MODEL), 1.0),
        'norm_mix': gain((L, D_MODEL)),
        'w_in': nrm((L, D_MODEL, IN_WIDTH), D_MODEL ** -0.5),
        'gmlp_ln_g': gain((L, GMLP_WIDTH)),
        'gmlp_ln_b': nrm((L, GMLP_WIDTH), 0.02),
        'gmlp_w_s': nrm((L, GMLP_GROUPS, GMLP_CHUNK, GMLP_CHUNK), GMLP_CHUNK ** -0.5),
        'gmlp_b_s': gain((L, GMLP_GROUPS, GMLP_CHUNK)),
        'pool_w': nrm((L, len(POOL_WINDOWS), POOL_GROUP, POOL_GROUP), POOL_GROUP ** -0.5),
        'pool_b': nrm((L, len(POOL_WINDOWS), POOL_GROUP), 0.02),
        'pool_scale': gain((L, POOL_WIDTH)),
        'conv_w': nrm((L, CONV_KERNEL, CONV_WIDTH), CONV_KERNEL ** -0.5),
        'conv_b': nrm((L, CONV_WIDTH), 0.02),
        'conv_ln_g': gain((L, CONV_WIDTH)),
        'conv_ln_b': nrm((L, CONV_WIDTH), 0.02),
        'w_br_a': nrm((L, GMLP_WIDTH, D_MODEL), GMLP_WIDTH ** -0.5),
        'w_br_b': nrm((L, POOL_WIDTH, D_MODEL), POOL_WIDTH ** -0.5),
        'w_br_c': nrm((L, ATTN_WIDTH, D_MODEL), ATTN_WIDTH ** -0.5),
        'w_br_d': nrm((L, CONV_WIDTH, D_MODEL), CONV_WIDTH ** -0.5),
        'w_gate': nrm((L, D_MODEL, N_BRANCHES * D_MODEL), D_MODEL ** -0.5),
        'b_gate': nrm((L, N_BRANCHES * D_MODEL), 0.02),
        'w_out': nrm((L, D_MODEL, D_MODEL), D_MODEL ** -0.5),
        'norm_ffn': gain((L, D_MODEL)),
        'dense_w1': nrm((N_DENSE, D_MODEL, D_FF_DENSE), D_MODEL ** -0.5),
        'dense_w3': nrm((N_DENSE, D_MODEL, D_FF_DENSE), D_MODEL ** -0.5),
        'dense_w2': nrm((N_DENSE, D_FF_DENSE, D_MODEL), D_FF_DENSE ** -0.5),
        'moe_router': nrm((N_MOE, D_MODEL, N_EXPERTS), D_MODEL ** -0.5),
        'moe_w1': nrm((N_MOE, N_EXPERTS, D_MODEL, D_FF_EXPERT), D_MODEL ** -0.5),
        'moe_w3': nrm((N_MOE, N_EXPERTS, D_MODEL, D_FF_EXPERT), D_MODEL ** -0.5),
        'moe_w2': nrm((N_MOE, N_EXPERTS, D_FF_EXPERT, D_MODEL), D_FF_EXPERT ** -0.5),
        'norm_final': gain((D_MODEL,)),
    }


def reference(x, norm_mix, w_in, gmlp_ln_g, gmlp_ln_b, gmlp_w_s, gmlp_b_s, pool_w, pool_b, pool_scale,
              conv_w, conv_b, conv_ln_g, conv_ln_b, w_br_a, w_br_b, w_br_c, w_br_d, w_gate, b_gate, w_out,
              norm_ffn, dense_w1, dense_w3, dense_w2, moe_router, moe_w1, moe_w3, moe_w2, norm_final):
    b, s, d = x.shape
    slopes = alibi_slopes(ATTN_HEADS)
    for l in range(DEPTH):
        h = rmsnorm(x, norm_mix[l])
        z = h @ w_in[l]
        z_a, z_b, z_q, z_k, z_v, z_d = jnp.split(z, IN_SPLITS, axis=-1)
        y_a = gmlp_spatial_gating(jax.nn.gelu(z_a), gmlp_ln_g[l], gmlp_ln_b[l], gmlp_w_s[l], gmlp_b_s[l])
        y_b = multiscale_pool(z_b, pool_w[l], pool_b[l], pool_scale[l])
        q = z_q.reshape(b, s, ATTN_HEADS, HEAD_DIM)
        k = z_k.reshape(b, s, ATTN_HEADS, HEAD_DIM)
        v = z_v.reshape(b, s, ATTN_HEADS, HEAD_DIM)
        y_c = dilated_mixture_attention(q, k, v, slopes).reshape(b, s, ATTN_WIDTH)
        y_d = conformer_conv(z_d, conv_w[l], conv_b[l], conv_ln_g[l], conv_ln_b[l])
        gates = jax.nn.sigmoid(h @ w_gate[l] + b_gate[l]).reshape(b, s, N_BRANCHES, d)
        merged = (gates[:, :, 0] * (y_a @ w_br_a[l]) + gates[:, :, 1] * (y_b @ w_br_b[l])
                  + gates[:, :, 2] * (y_c @ w_br_c[l]) + gates[:, :, 3] * (y_d @ w_br_d[l]))
        x = x + (merged @ w_out[l]).astype(x.dtype)
        h = rmsnorm(x, norm_ffn[l])
        i = l // 2
        if l % 2 == 0:
            f = swiglu(h, dense_w1[i], dense_w3[i], dense_w2[i])
        else:
            f = moe_swiglu(h, moe_router[i], moe_w1[i], moe_w3[i], moe_w2[i])
        x = x + f.astype(x.dtype)
    return rmsnorm(x, norm_final)
```

```python
import numpy as np
import ml_dtypes
import concourse.bass as bass
import concourse.mybir as mybir
from concourse.bass_utils import run_bass_kernel_spmd

F32 = mybir.dt.float32
BF16 = mybir.dt.bfloat16
AF = mybir.ActivationFunctionType
ALU = mybir.AluOpType
AX = mybir.AxisListType

NCORES = 8
S = 2048
TT = 512
NT = S // TT
NCH = S // 128
D = 1024
KC = 8
DFF = 2816
DFE = 3584
NEXP = 8
INW = 2816
RMS_EPS = 1e-6
LN_EPS = 1e-5
ENGS = ["pe", "act", "dve", "pool", "sp"]

VL = {}
_o = 0
for _l in range(2):
    for _n, _w in (("nmix", 8), ("nffn", 8), ("bgate", 32), ("poolb", 2), ("pools", 2), ("convw", 62),
                   ("convb", 2), ("clng", 2), ("clnb", 2)):
        VL[(_n, _l)] = _o
        _o += _w
VL["nfinal"] = _o; _o += 8
VL["pinvw"] = _o; _o += 2
VL["pinv16"] = _o; _o += 32
NV = _o
NROW = 2048


class Buf:
    __slots__ = ("name", "w", "rd", "rd_dma")

    def __init__(self, name):
        self.name = name
        self.w = None
        self.rd = {}
        self.rd_dma = []


class Op:
    __slots__ = ("eng", "fn", "deps", "signal", "is_dma", "dma_key", "dma_cnt", "sig_k", "group")


class Prog:
    R = 8

    def __init__(self):
        self.ops = {e: [] for e in ENGS}
        self.dma_cnt = {}
        self.last_dma = {}
        self.pending = {e: [] for e in ENGS}
        self.cur_group = None
        self.ngroups = 0

    def cond_begin(self, thr):
        self.ngroups += 1
        self.cur_group = (self.ngroups, thr)

    def cond_end(self):
        self.cur_group = None

    def emit(self, eng, fn, reads=(), writes=(), dma_key=None):
        op = Op()
        op.eng = eng; op.fn = fn; op.signal = False
        op.is_dma = dma_key is not None
        op.dma_key = dma_key; op.dma_cnt = 0; op.sig_k = -1
        op.group = self.cur_group
        deps = {}

        def add(d, kind):
            if d is None:
                return
            if (not d.is_dma) and d.eng == eng:
                if kind not in ("raw", "waw") or eng == "pe":
                    return
            deps[id(d)] = d

        for b in reads:
            add(b.w, "raw")
        for b in writes:
            add(b.w, "waw")
            for r in b.rd.values():
                add(r, "war")
            for r in b.rd_dma:
                add(r, "war")
        for d in self.pending[eng]:
            add(d, "bar")
        self.pending[eng] = []
        for b in reads:
            if op.is_dma:
                b.rd_dma.append(op)
            else:
                b.rd[eng] = op
        for b in writes:
            b.w = op
            b.rd = {}
            b.rd_dma = []
        for d in deps.values():
            d.signal = True
        op.deps = list(deps.values())
        if op.is_dma:
            c = self.dma_cnt.get(dma_key, 0) + 1
            self.dma_cnt[dma_key] = c
            op.dma_cnt = c
            self.last_dma[dma_key] = op
        self.ops[eng].append(op)
        return op

    def barrier(self):
        lst = []
        for e in ENGS:
            for op in reversed(self.ops[e]):
                if not op.is_dma:
                    lst.append(op)
                    break
        lst += list(self.last_dma.values())
        for e in ENGS:
            self.pending[e] = list(lst)

    def finalize(self, nc, stack):
        R = self.R
        sems = {}
        for e in ("pe", "act", "dve", "pool", "sp"):
            k = 0
            for op in self.ops[e]:
                if op.signal and not op.is_dma:
                    op.sig_k = k
                    k += 1
            if k > 0:
                sems[e] = [stack.enter_context(nc.semaphore(f"s_{e}_{i}")) for i in range(min(R, k))]
        dsem = {key: stack.enter_context(nc.semaphore(f"d_{i}")) for i, key in enumerate(self.dma_cnt)}
        block = stack.enter_context(nc.Block())
        prog = self

        ENG_OBJ = {"pe": nc.tensor, "act": nc.scalar, "dve": nc.vector, "pool": nc.gpsimd, "sp": nc.sync}
        use_groups = any(op.group is not None for e_ in ENGS for op in prog.ops[e_])
        REG = {}
        if use_groups:
            for e_ in ENGS:
                REG[e_] = stack.enter_context(ENG_OBJ[e_].register(f"ncnt_{e_}"))
        prog.REG = REG

        def run(eng, e):
            waited = {}
            waited_d = {}
            last_sig = [-1]

            def emit_op(op):
                for d in op.deps:
                    if d.is_dma:
                        if waited_d.get(d.dma_key, 0) >= d.dma_cnt:
                            continue
                        e.wait_ge(dsem[d.dma_key], 16 * d.dma_cnt)
                        waited_d[d.dma_key] = d.dma_cnt
                    else:
                        if waited.get(d.eng, -1) >= d.sig_k:
                            continue
                        e.wait_ge(sems[d.eng][d.sig_k % R], d.sig_k // R + 1)
                        waited[d.eng] = d.sig_k
                ins = op.fn(e)
                if op.is_dma:
                    ins.then_inc(dsem[op.dma_key], 16)
                elif op.signal:
                    ins.then_inc(sems[eng][op.sig_k % R], 1)
                    last_sig[0] = op.sig_k

            ops = prog.ops[eng]
            i = 0
            while i < len(ops):
                op = ops[i]
                if op.group is None:
                    emit_op(op)
                    i += 1
                    continue
                j = i
                while j < len(ops) and ops[j].group == op.group:
                    j += 1
                grp = ops[i:j]
                thr = op.group[1]
                sv_w, sv_d, sv_ls = dict(waited), dict(waited_d), last_sig[0]
                with e.If_lt(REG[eng], thr + 1):
                    if last_sig[0] >= 0:
                        k = last_sig[0]
                        e.wait_ge(sems[eng][k % R], k // R + 1)
                    cnts = {}
                    dk = {}
                    for g in grp:
                        if g.is_dma:
                            if g.dma_key not in dk:
                                dk[g.dma_key] = [g.dma_cnt - 1, 0]
                            dk[g.dma_key][1] += 1
                        elif g.signal:
                            cnts[g.sig_k % R] = cnts.get(g.sig_k % R, 0) + 1
                    for si_, c_ in cnts.items():
                        e.sem_inc(sems[eng][si_], c_)
                    for key, (before, n_) in dk.items():
                        if before > 0:
                            e.wait_ge(dsem[key], 16 * before)
                        e.sem_inc(dsem[key], 16 * n_)
                with e.Else():
                    for g in grp:
                        emit_op(g)
                waited.clear(); waited.update(sv_w)
                waited_d.clear(); waited_d.update(sv_d)
                i = j
            if eng == "sp":
                for key, c in prog.dma_cnt.items():
                    e.wait_ge(dsem[key], 16 * c)

        @block.tensor
        def _(e):
            run("pe", e)

        @block.scalar
        def _(e):
            run("act", e)

        @block.vector
        def _(e):
            run("dve", e)

        @block.gpsimd
        def _(e):
            run("pool", e)

        @block.sync
        def _(e):
            run("sp", e)


class Arena:
    def __init__(self, ap, n):
        self.ap = ap; self.n = n; self.off = 0; self.marks = []; self.peak = 0

    def alloc(self, free, dt):
        n = 1
        for f in free:
            n *= f
        nb = n * (2 if dt is F32 else 1)
        off = (self.off + 63) // 64 * 64
        assert off + nb <= self.n, f"arena overflow {off + nb} > {self.n}"
        v = self.ap[:, off:off + nb]
        if dt is F32:
            v = v.bitcast(F32)
        if len(free) == 2:
            v = v.rearrange("p (a b) -> p a b", a=free[0])
        elif len(free) == 3:
            v = v.rearrange("p (a b c) -> p a b c", a=free[0], b=free[1])
        self.off = off + nb
        self.peak = max(self.peak, self.off)
        return v

    def mark(self):
        self.marks.append(self.off)

    def release(self):
        self.off = self.marks.pop()


def bufs(name, *dims):
    if len(dims) == 0:
        return Buf(name)
    return [bufs(f"{name}_{i}", *dims[1:]) for i in range(dims[0])]


ARENA_N = 105600
CAP = 2048
CFW = 128 * 4 + 1024 + 8
ROUTED = True


def build_program(nseq=2, stop_after=None, dbg=False):
    nc = bass.Bass("TRN2", target_bir_lowering=False)
    from contextlib import ExitStack
    stack = ExitStack()
    P = Prog()

    def dram(name, shape, dt=F32, kind="ExternalInput"):
        return nc.dram_tensor(name, list(shape), dt, kind=kind).ap()

    x_d = dram("xT", [nseq, KC, 128, S])
    out_d = dram("outT", [nseq, KC, 128, S], kind="ExternalOutput")
    vecs_d = dram("vecs", [128, NV])
    rows_d = dram("rows", [128, 2 * NROW])
    cf_d = dram("cf32", [128, CFW])
    cb_d = dram("cb16", [128, 256], BF16)
    hs_d = dram("hs_scratch", [NEXP * CAP, D], BF16, kind="Internal")
    ys_d = dram("ys_scratch", [NEXP * CAP, D], F32, kind="Internal")
    mtab_d = dram("mtab", [8, 128, 2048], BF16)
    w_in_d = dram("w_in", [2, D, INW])
    wst_d = dram("wst", [2, 128, 4, 128])
    pool_w_d = dram("pool_w", [2, 4, 64, 64])
    wbr_d = [dram("w_br_a", [2, 256, D]), dram("w_br_b", [2, 256, D]), dram("w_br_c", [2, 512, D]), dram("w_br_d", [2, 256, D])]
    w_gate_d = dram("w_gate", [2, D, 4 * D])
    w_out_d = dram("w_out", [2, D, D])
    dw1_d = dram("dense_w1", [1, D, DFF]); dw3_d = dram("dense_w3", [1, D, DFF]); dw2_d = dram("dense_w2", [1, DFF, D])
    router_d = dram("moe_router", [1, D, NEXP])
    mw1_d = dram("moe_w1", [1, NEXP, D, DFE]); mw3_d = dram("moe_w3", [1, NEXP, D, DFE]); mw2_d = dram("moe_w2", [1, NEXP, DFE, D])
    dbg_out = {}

    arena_t = stack.enter_context(nc.sbuf_tensor("arena", [128, ARENA_N], BF16))
    A = Arena(arena_t, ARENA_N)
    psb = [stack.enter_context(nc.psum_tensor(f"ps{i}", [128, 512], F32)) for i in range(8)]
    PS = [(psb[i][:, :], Buf(f"ps{i}")) for i in range(8)]
    rings = {"a": [0, 1], "b": [2, 3], "c": [4, 5], "d": [6, 7], "s": [0, 1, 2, 3, 6, 7]}
    rpos = {k: 0 for k in rings}

    def psum(ring):
        lst = rings[ring]
        i = lst[rpos[ring] % len(lst)]
        rpos[ring] += 1
        return PS[i]

    def mm(out, lhsT, rhs, start, stop, reads, writes):
        P.emit("pe", lambda e: e.matmul(out, lhsT, rhs, start=start, stop=stop), reads, writes)

    def act(out, in_, func, reads, writes, bias=None, scale=None):
        kw = {}
        if bias is not None:
            kw["bias"] = bias
        if scale is not None:
            kw["scale"] = scale
        P.emit("act", lambda e: e.activation(out=out, in_=in_, func=func, **kw), reads, writes)

    def tt_(out, in0, in1, op, reads, writes):
        P.emit("dve", lambda e: e.tensor_tensor(out=out, in0=in0, in1=in1, op=op), reads, writes)

    def ts_(out, in0, s1, s2, op0, op1, reads, writes):
        if op1 is None:
            P.emit("dve", lambda e: e.tensor_scalar(out=out, in0=in0, scalar1=s1, scalar2=None, op0=op0), reads, writes)
        else:
            P.emit("dve", lambda e: e.tensor_scalar(out=out, in0=in0, scalar1=s1, scalar2=s2, op0=op0, op1=op1), reads, writes)

    def stt_(out, in0, scalar, in1, op0, op1, reads, writes):
        P.emit("dve", lambda e: e.scalar_tensor_tensor(out=out, in0=in0, scalar=scalar, in1=in1, op0=op0, op1=op1), reads, writes)

    def dcopy(out, in_, reads, writes):
        P.emit("dve", lambda e: e.tensor_copy(out=out, in_=in_), reads, writes)

    def dma(q, out, in_, reads, writes, key):
        P.emit(q, lambda e: e.dma_start(out=out, in_=in_), reads, writes, dma_key=key)

    def memset(ap, val, writes):
        P.emit("dve", lambda e: e.memset(ap, val), (), writes)

    def debug_dump(name, view, shape, dt, rbufs):
        if not dbg:
            return
        d = dram("dbg_" + name, shape, dt, kind="ExternalOutput")
        dbg_out[name] = 1
        dma("sp", d, view, rbufs, (), "dbg_" + name)

    xT = A.alloc((KC, S), F32); xT_b = bufs("xT", KC, NT)
    cf = A.alloc((CFW,), F32); cf_b = Buf("cf")
    offs = cf[:, 1536:1544]
    cb16 = A.alloc((256,), BF16); cb_b = Buf("cb16")
    ustrict = cb16[:, 0:128]; ones_b16 = cb16[:, 128:256]
    ident_b = A.alloc((128,), BF16); identb_b = Buf("identb")
    hs_bs = bufs("hs", NCH); ys_bs = bufs("ys", NEXP * 4)
    ident = cf[:, 0:128]; ones_f = cf[:, 128:256]; c256 = cf[:, 256:384]; cmask = cf[:, 384:512]; sel = cf[:, 512:1536]
    vecs = A.alloc((NV,), F32); vecs_b = Buf("vecs")
    rows_b = Buf("rows")
    rstd = A.alloc((S,), F32); rstd_b = bufs("rstd", NT)
    dma("sp", cf, cf_d, (), [cf_b], "cf")
    dma("sp", cb16, cb_d, (), [cb_b], "cb16")
    dcopy(ident_b, ident, [cf_b], [identb_b])
    dma("sp", vecs, vecs_d, (), [vecs_b], "vecs")

    def vcol(key, j=0, n=1):
        o = VL[key] + j
        return vecs[:, o:o + n]

    def flat(*ls):
        r = []
        for l in ls:
            if isinstance(l, Buf):
                r.append(l)
            else:
                r.extend(flat(*l))
        return r

    def rmsnorm_to_h(gkey, hT, hT_b, sqtmp, sq_b):
        for tt in range(NT):
            sl = slice(tt * TT, (tt + 1) * TT)
            ps, pb = psum("a")
            for c in range(KC):
                sq, sqb = sqtmp[c % 2], sq_b[c % 2]
                act(sq, xT[:, c, sl], AF.Square, [xT_b[c][tt]], [sqb])
                mm(ps, ones_f, sq, c == 0, c == KC - 1, [sqb, cf_b], [pb])
            act(rstd[:, sl], ps, AF.Sqrt, [pb, eps_b], [rstd_b[tt]], bias=eps_rms, scale=1.0 / D)
            P.emit("dve", lambda e, o=rstd[:, sl]: e.reciprocal(out=o, in_=o), [rstd_b[tt]], [rstd_b[tt]])
            if hT is not None:
                for c in range(KC):
                    stt_(hT[:, c, sl], xT[:, c, sl], vcol(gkey, c), rstd[:, sl], ALU.mult, ALU.mult,
                         [xT_b[c][tt], rstd_b[tt], vecs_b], [hT_b[c][tt]])

    eps_t = A.alloc((4,), F32); eps_b = Buf("eps")
    memset(eps_t[:, 0:1], RMS_EPS, [eps_b])
    memset(eps_t[:, 1:2], LN_EPS, [eps_b])
    eps_rms = eps_t[:, 0:1]; eps_ln = eps_t[:, 1:2]

    def wload(slot, slot_b, w2d, c0, n, kchunks, key, k0=0):
        src = w2d.rearrange("(c p) n -> p c n", p=128)[:, k0:k0 + kchunks, c0:c0 + n]
        dma("pool", slot[:, 0:kchunks, 0:n], src, (), [slot_b], key)

    def proj_fm(ps, pb, w, wb, col, hT, hT_b, tt, kchunks=KC, koff=0, width=TT, t0=None):
        if t0 is None:
            t0 = tt * TT
        for k in range(kchunks):
            mm(ps[:, 0:width], w[:, k, col:col + 128], hT[:, koff + k, t0:t0 + width], k == 0, k == kchunks - 1,
               [wb, hT_b[koff + k][tt]], [pb])

    for sq_i in range(nseq):
        for c in range(KC):
            dma("sp", xT[:, c, :], x_d[sq_i, c], (), xT_b[c], f"x{c}")
        for l in range(2):
            P.barrier()
            A.mark()
            hT = A.alloc((KC, S), BF16); hT_b = bufs("hT", KC, NT)
            yT = A.alloc((10, S), BF16); yT_b = bufs("yT", 10, NT)
            A.mark()
            sqtmp = [A.alloc((TT,), F32) for _ in range(2)]; sq_b = bufs("sq", 2)
            rmsnorm_to_h(("nmix", l), hT, hT_b, sqtmp, sq_b)
            A.release()
            if dbg and sq_i == 0:
                debug_dump(f"h_{l}", hT, [128, KC, S], BF16, flat(hT_b))
            w_in_l = w_in_d[l]

            P.barrier(); A.mark()
            ws = A.alloc((KC, 512), BF16); ws_b = Buf("ws_d")
            wload(ws, ws_b, w_in_l, 2304, 512, KC, "ws_d")
            ypad = [A.alloc((2, 30 + TT), BF16) for _ in range(2)]; ypad_b = bufs("ypad", 2, 2)
            cacc = A.alloc((2, TT), F32); cacc_b = bufs("cacc", 2)
            sg = [A.alloc((TT,), F32) for _ in range(2)]; sg_b = bufs("sg", 2)
            lnt = [A.alloc((TT,), F32) for _ in range(3)]; lnt_b = bufs("lnt", 3)
            dg = A.alloc((62, 128), BF16); dg_b = Buf("dg")
            for j in range(2):
                cw0 = VL[("convw", l)] + j * 31
                for k in range(31):
                    ts_(dg[:, j * 31 + k, :], ident, vecs[:, cw0 + k:cw0 + k + 1], None, ALU.mult, None, [cf_b, vecs_b], [dg_b])
            for i in range(2):
                for j in range(2):
                    memset(ypad[i][:, j, 0:30], 0.0, [ypad_b[i][j]])
            def d_front(tt):
                yp, ypb = ypad[tt % 2], ypad_b[tt % 2]
                if tt > 0:
                    ypp, yppb = ypad[(tt - 1) % 2], ypad_b[(tt - 1) % 2]
                    for j in range(2):
                        dcopy(yp[:, j, 0:30], ypp[:, j, TT:TT + 30], [yppb[j]], [ypb[j]])
                for j in range(2):
                    pa, pab = psum("a")
                    proj_fm(pa, pab, ws, ws_b, j * 128, hT, hT_b, tt)
                    pg, pgb = psum("b")
                    proj_fm(pg, pgb, ws, ws_b, 256 + j * 128, hT, hT_b, tt)
                    act(sg[j], pg, AF.Sigmoid, [pgb], [sg_b[j]])
                    tt_(yp[:, j, 30:30 + TT], pa, sg[j], ALU.mult, [pab, sg_b[j]], [ypb[j]])

            def d_back(tt):
                sl = slice(tt * TT, (tt + 1) * TT)
                yp, ypb = ypad[tt % 2], ypad_b[tt % 2]
                for j in range(2):
                    pc, pcb = psum("c" if j == 0 else "d")
                    for k in range(31):
                        mm(pc, dg[:, j * 31 + k, :], yp[:, j, k:k + TT], k == 0, k == 30, [dg_b, ypb[j]], [pcb])
                    act(cacc[:, j, :], pc, AF.Identity, [pcb, vecs_b], [cacc_b[j]], bias=vcol(("convb", l), j))
                mean, var, rs = lnt[0], lnt[1], lnt[2]
                pm, pmb = psum("c")
                for j in range(2):
                    mm(pm, c256, cacc[:, j, :], j == 0, j == 1, [cacc_b[j], cf_b], [pmb])
                pq, pqb = psum("d")
                for j in range(2):
                    act(var, cacc[:, j, :], AF.Square, [cacc_b[j]], [lnt_b[1]])
                    mm(pq, c256, var, j == 0, j == 1, [lnt_b[1], cf_b], [pqb])
                dcopy(mean, pm, [pmb], [lnt_b[0]])
                tt_(var, mean, mean, ALU.mult, [lnt_b[0]], [lnt_b[1]])
                tt_(var, pq, var, ALU.subtract, [pqb, lnt_b[1]], [lnt_b[1]])
                act(rs, var, AF.Sqrt, [lnt_b[1], eps_b], [lnt_b[2]], bias=eps_ln)
                P.emit("dve", lambda e, o=rs: e.reciprocal(out=o, in_=o), [lnt_b[2]], [lnt_b[2]])
                for j in range(2):
                    tt_(cacc[:, j, :], cacc[:, j, :], mean, ALU.subtract, [cacc_b[j], lnt_b[0]], [cacc_b[j]])
                    tt_(cacc[:, j, :], cacc[:, j, :], rs, ALU.mult, [cacc_b[j], lnt_b[2]], [cacc_b[j]])
                    act(yT[:, 8 + j, sl], cacc[:, j, :], AF.Silu, [cacc_b[j], vecs_b], [yT_b[8 + j][tt]],
                        bias=vcol(("clnb", l), j), scale=vcol(("clng", l), j))

            d_front(0)
            for tt in range(NT):
                if tt + 1 < NT:
                    d_front(tt + 1)
                d_back(tt)
            A.release()

            P.barrier(); A.mark()
            ws = A.alloc((KC, 256), BF16); ws_b = Buf("ws_b")
            wload(ws, ws_b, w_in_l, 512, 256, KC, "ws_b")
            pwd = A.alloc((2, 128), BF16); pwd_b = Buf("pwd")
            memset(pwd, 0.0, [pwd_b])
            for g in range(4):
                j, hp = g // 2, g % 2
                dma("pool", pwd[hp * 64:(hp + 1) * 64, j, hp * 64:(hp + 1) * 64], pool_w_d[l, g], (), [pwd_b], "pwd")
            zp = [A.alloc((2, 16 + TT), F32) for _ in range(2)]; zp_b = bufs("zp", 2, 2)
            sA = A.alloc((16 + TT,), F32); sB = A.alloc((16 + TT,), F32); sAB_b = bufs("sAB", 2)
            pl = A.alloc((TT,), F32); pl_b = Buf("pl")
            plb = A.alloc((TT,), BF16); plb_b = Buf("plb")
            for i in range(2):
                for j in range(2):
                    memset(zp[i][:, j, 0:16], 0.0, [zp_b[i][j]])
            def b_front(tt):
                z, zb = zp[tt % 2], zp_b[tt % 2]
                if tt > 0:
                    zpp, zppb = zp[(tt - 1) % 2], zp_b[(tt - 1) % 2]
                    for j in range(2):
                        dcopy(z[:, j, 0:16], zpp[:, j, TT:TT + 16], [zppb[j]], [zb[j]])
                for j in range(2):
                    pz, pzb = psum("a")
                    proj_fm(pz, pzb, ws, ws_b, j * 128, hT, hT_b, tt)
                    dcopy(z[:, j, 16:16 + TT], pz, [pzb], [zb[j]])

            def b_back(tt):
                sl = slice(tt * TT, (tt + 1) * TT)
                z, zb = zp[tt % 2], zp_b[tt % 2]
                for j in range(2):
                    W = 16 + TT
                    tt_(sA[:, 1:W], z[:, j, 1:W], z[:, j, 0:W - 1], ALU.add, [zb[j]], [sAB_b[0]])
                    if j == 0:
                        tt_(sB[:, 3:W], sA[:, 3:W], sA[:, 1:W - 2], ALU.add, [sAB_b[0]], [sAB_b[1]])
                        lo, hi = sA, sB
                        lob, hib = sAB_b[0], sAB_b[1]
                    else:
                        tt_(sB[:, 3:W], sA[:, 3:W], sA[:, 1:W - 2], ALU.add, [sAB_b[0]], [sAB_b[1]])
                        tt_(sA[:, 7:W], sB[:, 7:W], sB[:, 3:W - 4], ALU.add, [sAB_b[1]], [sAB_b[0]])
                        tt_(sB[:, 15:W], sA[:, 15:W], sA[:, 7:W - 8], ALU.add, [sAB_b[0]], [sAB_b[1]])
                        lo, hi = sA, sB
                        lob, hib = sAB_b[0], sAB_b[1]
                    ivw = vecs[:, VL["pinvw"] + j:VL["pinvw"] + j + 1]
                    for (src, srcb, p0) in ((lo, lob, 0), (hi, hib, 64)):
                        ps_ = slice(p0, p0 + 64)
                        stt_(pl[ps_, :], src[ps_, 16:W], ivw[ps_, :], z[ps_, j, 16:W], ALU.mult, ALU.subtract,
                             [srcb, zb[j], vecs_b], [pl_b])
                    if tt == 0:
                        o16 = VL["pinv16"] + j * 16
                        for (src, srcb, p0) in ((lo, lob, 0), (hi, hib, 64)):
                            ps_ = slice(p0, p0 + 64)
                            tt_(pl[ps_, 0:16], src[ps_, 16:32], vecs[ps_, o16:o16 + 16], ALU.mult, [srcb, vecs_b], [pl_b])
                            tt_(pl[ps_, 0:16], pl[ps_, 0:16], z[ps_, j, 16:32], ALU.subtract, [pl_b, zb[j]], [pl_b])
                    dcopy(plb, pl, [pl_b], [plb_b])
                    po, pob = psum("b")
                    mm(po, pwd[:, j, :], plb, True, True, [pwd_b, plb_b], [pob])
                    ts_(yT[:, 2 + j, sl], po, vcol(("poolb", l), j), vcol(("pools", l), j), ALU.add, ALU.mult,
                        [pob, vecs_b], [yT_b[2 + j][tt]])

            b_front(0)
            for tt in range(NT):
                if tt + 1 < NT:
                    b_front(tt + 1)
                b_back(tt)
            A.release()

            P.barrier(); A.mark()
            rows = A.alloc((NROW,), F32)
            dma("sp", rows, rows_d[:, l * NROW:(l + 1) * NROW], (), [rows_b], "rows")
            ws = A.alloc((KC, 512), BF16); ws_b = Buf("ws_a")
            wload(ws, ws_b, w_in_l, 0, 512, KC, "ws_a")
            wsf = A.alloc((4, 128), F32); wsf_b = Buf("wsf")
            wsT = A.alloc((4, 128), BF16); wsT_b = Buf("wsT")
            dma("sp", wsf, wst_d[l], (), [wsf_b], "wsf")
            for g in range(4):
                tt_(wsT[:, g, :], wsf[:, g, :], cmask, ALU.mult, [wsf_b, cf_b], [wsT_b])
            uT = [A.alloc((2, TT), F32) for _ in range(2)]; uT_b = bufs("uT", 2, 2)
            vg = [A.alloc((256,), F32) for _ in range(2)]; vg_b = bufs("vg", 2)
            vln = [A.alloc((4, 256), BF16) for _ in range(2)]; vln_b = bufs("vln", 2, 4)
            st6 = A.alloc((8,), F32); st_b = Buf("st6")
            mv = A.alloc((4,), F32); mv_b = Buf("mv")
            ytmp = [A.alloc((128,), F32) for _ in range(2)]; ytmp_b = bufs("ytmp", 2)
            r0 = 0
            lng = rows[:, r0:r0 + 256]; lnb = rows[:, r0 + 256:r0 + 512]
            def a_front(tt):
                u, ub = uT[tt % 2], uT_b[tt % 2]
                vl, vlb = vln[tt % 2], vln_b[tt % 2]
                for j in range(2):
                    pu, pub = psum("a")
                    proj_fm(pu, pub, ws, ws_b, j * 128, hT, hT_b, tt)
                    act(u[:, j, :], pu, AF.Gelu_apprx_tanh, [pub], [ub[j]])
                for ci in range(4):
                    ch = tt * 4 + ci
                    pv, pvb = psum("b")
                    for k in range(KC):
                        mm(pv[:, 0:256], hT[:, k, ch * 128:(ch + 1) * 128], ws[:, k, 256:512], k == 0, k == KC - 1,
                           [hT_b[k][tt], ws_b], [pvb])
                    v_, v_b = vg[ci % 2], vg_b[ci % 2]
                    act(v_, pv[:, 0:256], AF.Gelu_apprx_tanh, [pvb], [v_b])
                    P.emit("dve", lambda e, o=st6[:, 0:6], i=v_: e.bn_stats(out=o, in_=i), [v_b], [st_b])
                    P.emit("dve", lambda e, o=mv[:, 0:2], i=st6[:, 0:6]: e.bn_aggr(out=o, in_=i), [st_b], [mv_b])
                    act(mv[:, 2:3], mv[:, 1:2], AF.Sqrt, [mv_b, eps_b], [mv_b], bias=eps_ln)
                    P.emit("dve", lambda e, o=mv[:, 3:4], i=mv[:, 2:3]: e.reciprocal(out=o, in_=i), [mv_b], [mv_b])
                    ts_(v_, v_, mv[:, 0:1], mv[:, 3:4], ALU.subtract, ALU.mult, [v_b, mv_b], [v_b])
                    tt_(v_, v_, lng, ALU.mult, [v_b, rows_b], [v_b])
                    tt_(vl[:, ci, :], v_, lnb, ALU.add, [v_b, rows_b], [vlb[ci]])

            def a_back(tt):
                u, ub = uT[tt % 2], uT_b[tt % 2]
                vl, vlb = vln[tt % 2], vln_b[tt % 2]
                for j in range(2):
                    for gl in range(2):
                        g = 2 * j + gl
                        pm_, pmb_ = psum("c")
                        for ci in range(4):
                            mm(pm_[:, ci * 128:(ci + 1) * 128], vl[:, ci, j * 128:(j + 1) * 128], wsT[:, g, :], True, True,
                               [vlb[ci], wsT_b], [pmb_])
                        ps_ = slice(gl * 64, gl * 64 + 64)
                        bs = rows[:, r0 + 512 + g * 128:r0 + 512 + (g + 1) * 128]
                        for ci in range(4):
                            yt, ytb = ytmp[ci % 2], ytmp_b[ci % 2]
                            tt_(yt[ps_, :], pm_[ps_, ci * 128:(ci + 1) * 128], bs[ps_, :], ALU.add, [pmb_, rows_b], [ytb])
                            t0 = tt * TT + ci * 128
                            tt_(yT[ps_, j, t0:t0 + 128], yt[ps_, :], u[ps_, j, ci * 128:(ci + 1) * 128], ALU.mult,
                                [ytb, ub[j]], [yT_b[j][tt]])

            a_front(0)
            for tt in range(NT):
                if tt + 1 < NT:
                    a_front(tt + 1)
                a_back(tt)
            A.release()

            P.barrier(); A.mark()
            wqs = [A.alloc((KC, 384), BF16) for _ in range(2)]; wqs_b = bufs("wq", 2, 3)

            def load_wq(jp_):
                for wi, c0 in enumerate((768, 1280, 1792)):
                    src = w_in_l.rearrange("(c p) n -> p c n", p=128)[:, :, c0 + jp_ * 128:c0 + (jp_ + 1) * 128]
                    dma("pool", wqs[jp_ % 2][:, :, wi * 128:(wi + 1) * 128], src, (), [wqs_b[jp_ % 2][wi]], f"wq{jp_ % 2}{wi}")

            load_wq(0)
            qT = A.alloc((S,), BF16); qT_b = bufs("qT", NT)
            kT = A.alloc((S,), BF16); kT_b = bufs("kT", NT)
            vaug = A.alloc((NCH, 2, 128), BF16); vaug_b = bufs("vaug", NT)
            NMT = 2
            NPT = 6
            mt = [A.alloc((2048,), BF16) for _ in range(NMT)]; mt_b = bufs("mt", NMT)
            pts = [A.alloc((TT,), BF16) for _ in range(NPT)]; pts_b = bufs("pts", NPT)
            rec = [A.alloc((TT,), F32) for _ in range(2)]; rec_b = bufs("rec", 2)
            memset(vaug[:, :, :, 64:128], 1.0, flat(vaug_b))
            pti = [0]
            for jp in range(4):
                wq, wq_b = wqs[jp % 2], wqs_b[jp % 2]
                if jp + 1 < 4:
                    load_wq(jp + 1)
                for tt in range(NT):
                    sl = slice(tt * TT, (tt + 1) * TT)
                    pq_, pqb_ = psum("a")
                    for k in range(KC):
                        mm(pq_, wq[:, k, 0:128], hT[:, k, sl], k == 0, k == KC - 1, [wq_b[0], hT_b[k][tt]], [pqb_])
                    act(qT[:, sl], pq_, AF.Copy, [pqb_], [qT_b[tt]], scale=0.125)
                    pk_, pkb_ = psum("a")
                    for k in range(KC):
                        mm(pk_, wq[:, k, 128:256], hT[:, k, sl], k == 0, k == KC - 1, [wq_b[1], hT_b[k][tt]], [pkb_])
                    dcopy(kT[:, sl], pk_, [pkb_], [kT_b[tt]])
                    pv_, pvb_ = psum("b")
                    for ci in range(4):
                        ch = tt * 4 + ci
                        for k in range(KC):
                            mm(pv_[:, ci * 128:(ci + 1) * 128], hT[:, k, ch * 128:(ch + 1) * 128], wq[:, k, 256:384],
                               k == 0, k == KC - 1, [wq_b[2], hT_b[k][tt]], [pvb_])
                    for ci in range(4):
                        ch = tt * 4 + ci
                        for hh in range(2):
                            dcopy(vaug[:, ch, hh, 0:64], pv_[:, ci * 128 + hh * 64:ci * 128 + hh * 64 + 64], [pvb_], [vaug_b[tt]])
                steps = []
                for hh in range(2):
                    for tt in range(NT):
                        for J in range(4 * tt + 4):
                            steps.append((hh, tt, J, 4 * tt + 4))
                LA = 3
                st_state = {}
                po_state = {}

                def front(i):
                    hh, tt, J, nJ = steps[i]
                    h = jp * 2 + hh
                    hb = hh * 64
                    t0 = tt * TT
                    m_, m_b = mt[h % NMT], mt_b[h % NMT]
                    if tt == 0 and J == 0:
                        dma("sp", m_, mtab_d[h], (), [m_b], f"mt{h % NMT}")
                    col_lo = max(0, J * 128 - t0)
                    wd = TT - col_lo
                    ps_, psb_ = psum("s")
                    mm(ps_[:, 0:wd], kT[hb:hb + 64, J * 128:(J + 1) * 128], qT[hb:hb + 64, t0 + col_lo:t0 + TT], True, True,
                       [kT_b[J // 4], qT_b[tt]], [psb_])
                    pt, ptb = pts[pti[0] % NPT], pts_b[pti[0] % NPT]
                    pti[0] += 1
                    act(pt[:, 0:wd], ps_[:, 0:wd], AF.Exp, [psb_], [ptb])
                    o_first = max(4 * tt, J) - J
                    tt_(pt[:, 0:wd], pt[:, 0:wd], m_[:, o_first * 128:o_first * 128 + wd], ALU.mult, [ptb, m_b], [ptb])
                    st_state[i] = (pt, ptb, col_lo, wd)

                def back(i):
                    hh, tt, J, nJ = steps[i]
                    hb = hh * 64
                    t0 = tt * TT
                    pt, ptb, col_lo, wd = st_state.pop(i)
                    if J == 0:
                        po_state[(hh, tt)] = psum("c")
                    po_, pob_ = po_state[(hh, tt)]
                    mm(po_[:, col_lo:TT], vaug[:, J, hh, :], pt[:, 0:wd], J == 0, J == nJ - 1, [vaug_b[J // 4], ptb], [pob_])
                    if J == nJ - 1:
                        rc, rcb = rec[tt % 2], rec_b[tt % 2]
                        act(rc[0:64, :], po_[64:128, :], AF.Ln, [pob_], [rcb])
                        act(rc[0:64, :], rc[0:64, :], AF.Exp, [rcb], [rcb], scale=-1.0)
                        tt_(yT[hb:hb + 64, 4 + jp, t0:t0 + TT], po_[0:64, :], rc[0:64, :], ALU.mult, [pob_, rcb], [yT_b[4 + jp][tt]])

                for i in range(len(steps) + LA):
                    if i < len(steps):
                        front(i)
                    if i - LA >= 0:
                        back(i - LA)
            A.release()
            if dbg and sq_i == 0:
                debug_dump(f"y_{l}", yT, [128, 10, S], BF16, flat(yT_b))
            if stop_after == ("mixers", l):
                break

            P.barrier(); A.mark()
            wg = [A.alloc((KC, 256), BF16) for _ in range(4)]; wg_b = bufs("wg", 4)
            wbr = A.alloc((10, 256), BF16); wbr_b = bufs("wbr", 4)
            wo = A.alloc((2, D), BF16); wo_b = Buf("wo")
            mg = A.alloc((2, S), BF16); mg_b = bufs("mg", 2, NT)
            gt = [A.alloc((TT,), F32) for _ in range(2)]; gt_b = bufs("gt", 2)
            acc = [A.alloc((TT,), F32) for _ in range(2)]; acc_b = bufs("acc", 2)
            tmpm = [A.alloc((TT,), F32) for _ in range(2)]; tmpm_b = bufs("tmpm", 2)
            koffs = (0, 2, 4, 8); kcs = (2, 2, 4, 2)
            gti = 0; aci = 0; tmi = 0
            for fg in range(4):
                for i in range(4):
                    wload(wg[i], wg_b[i], w_gate_d[l], i * D + fg * 256, 256, KC, f"wg{i}")
                    src = wbr_d[i][l].rearrange("(c p) n -> p c n", p=128)[:, :, fg * 256:(fg + 1) * 256]
                    dma("pool", wbr[:, koffs[i]:koffs[i] + kcs[i], :], src, (), [wbr_b[i]], f"wbr{i}")
                src = w_out_d[l].rearrange("(c p) n -> p c n", p=128)[:, fg * 2:fg * 2 + 2, :]
                dma("pool", wo, src, (), [wo_b], "wo")
                for tt in range(NT):
                    sl = slice(tt * TT, (tt + 1) * TT)
                    for cc in range(2):
                        c = fg * 2 + cc
                        ac, acb = acc[aci % 2], acc_b[aci % 2]; aci += 1
                        for i in range(4):
                            pg_, pgb_ = psum("a")
                            proj_fm(pg_, pgb_, wg[i], wg_b[i], cc * 128, hT, hT_b, tt)
                            g_, g_b = gt[gti % 2], gt_b[gti % 2]; gti += 1
                            act(g_, pg_, AF.Sigmoid, [pgb_, vecs_b], [g_b], bias=vcol(("bgate", l), i * 8 + c))
                            pp_, ppb_ = psum("b")
                            for k in range(kcs[i]):
                                mm(pp_, wbr[:, koffs[i] + k, cc * 128:(cc + 1) * 128], yT[:, koffs[i] + k, sl], k == 0, k == kcs[i] - 1,
                                   [wbr_b[i], yT_b[koffs[i] + k][tt]], [ppb_])
                            if i == 0:
                                tt_(ac, pp_, g_, ALU.mult, [ppb_, g_b], [acb])
                            else:
                                tm, tmb = tmpm[tmi % 2], tmpm_b[tmi % 2]; tmi += 1
                                tt_(tm, pp_, g_, ALU.mult, [ppb_, g_b], [tmb])
                                if i < 3:
                                    tt_(ac, ac, tm, ALU.add, [acb, tmb], [acb])
                                else:
                                    tt_(mg[:, cc, sl], ac, tm, ALU.add, [acb, tmb], [mg_b[cc][tt]])
                for tt in range(NT):
                    sl = slice(tt * TT, (tt + 1) * TT)
                    for c2 in range(KC):
                        po_, pob_ = psum("c")
                        for cc in range(2):
                            mm(po_, wo[:, cc, c2 * 128:(c2 + 1) * 128], mg[:, cc, sl], cc == 0, cc == 1, [wo_b, mg_b[cc][tt]], [pob_])
                        tt_(xT[:, c2, sl], xT[:, c2, sl], po_, ALU.add, [xT_b[c2][tt], pob_], [xT_b[c2][tt]])
            A.release()
            A.release()
            if dbg and sq_i == 0:
                debug_dump(f"xmix_{l}", xT, [128, KC, S], F32, flat(xT_b))
            if stop_after == ("mix", l):
                break

            P.barrier(); A.mark()
            if l == 1 and ROUTED:
                I32 = mybir.dt.int32
                IDX = A.alloc((NCH, 2), F32).bitcast(I32); idx_b = bufs("idx", NCH)
                W12 = A.alloc((NCH, 2), F32); w12_b = bufs("w12", NCH)
                cnt_i = A.alloc((8,), F32).bitcast(I32); cnt_b = Buf("cnt")
                A.mark()
                hT = A.alloc((KC, S), BF16); hT_b = bufs("hT2", KC, NT)
                sqtmp = [A.alloc((TT,), F32) for _ in range(2)]; sq_b = bufs("sq", 2)
                rmsnorm_to_h(("nffn", l), hT, hT_b, sqtmp, sq_b)
                wr = A.alloc((KC, NEXP), F32); wr_b = Buf("wr")
                rt = A.alloc((96,), F32); rt_b = Buf("rt")
                selb = A.alloc((8,), BF16); selb_b = Buf("selb")
                selacc = A.alloc((8,), BF16); selacc_b = Buf("selacc")
                htok = [A.alloc((D,), BF16) for _ in range(2)]; htok_b = bufs("htok", 2)
                dma("sp", wr, router_d[0].rearrange("(c p) e -> p c e", p=128), (), [wr_b], "wr")
                for c in range(KC):
                    ts_(wr[:, c, :], wr[:, c, :], vcol(("nffn", l), c), None, ALU.mult, None, [wr_b, vecs_b], [wr_b])
                memset(selacc, 0.0, [selacc_b])
                for ch in range(NCH):
                    tt = ch // 4
                    csl = slice(ch * 128, (ch + 1) * 128)
                    pl_, plb_ = psum("a")
                    for c in range(KC):
                        mm(pl_[:, 0:NEXP], xT[:, c, csl], wr[:, c, :], c == 0, c == KC - 1, [xT_b[c][tt], wr_b], [plb_])
                    mm(pl_[:, 16:17], rstd[:, csl], c256[:, 0:1], True, True, [rstd_b[tt], cf_b], [plb_])
                    ts_(rt[:, 16:17], pl_[:, 16:17], 2.0, None, ALU.mult, None, [plb_], [rt_b])
                    ts_(rt[:, 0:8], pl_[:, 0:8], rt[:, 16:17], None, ALU.mult, None, [plb_, rt_b], [rt_b])
                    P.emit("dve", lambda e, o=rt[:, 8:16], i=rt[:, 0:8]: e.max(out=o, in_=i), [rt_b], [rt_b])
                    ts_(rt[:, 17:18], rt[:, 8:9], -1.0, None, ALU.mult, None, [rt_b], [rt_b])
                    ts_(rt[:, 24:32], rt[:, 0:8], rt[:, 9:10], None, ALU.is_ge, None, [rt_b], [rt_b])
                    act(rt[:, 32:40], rt[:, 0:8], AF.Exp, [rt_b], [rt_b], bias=rt[:, 17:18])
                    tt_(rt[:, 32:40], rt[:, 32:40], rt[:, 24:32], ALU.mult, [rt_b], [rt_b])
                    P.emit("dve", lambda e, o=rt[:, 40:41], i=rt[:, 32:40]: e.tensor_reduce(out=o, in_=i, axis=AX.X, op=ALU.add), [rt_b], [rt_b])
                    P.emit("dve", lambda e, o=rt[:, 41:42], i=rt[:, 40:41]: e.reciprocal(out=o, in_=i), [rt_b], [rt_b])
                    ts_(rt[:, 48:56], rt[:, 32:40], rt[:, 41:42], None, ALU.mult, None, [rt_b], [rt_b])
                    dcopy(selb, rt[:, 24:32], [rt_b], [selb_b])
                    pr_, prb_ = psum("b")
                    mm(pr_[:, 0:8], ustrict, selb, True, False, [cb_b, selb_b], [prb_])
                    mm(pr_[:, 0:8], ones_b16, selacc, False, True, [cb_b, selacc_b], [prb_])
                    tt_(rt[:, 56:64], pr_[:, 0:8], offs, ALU.add, [prb_, cf_b], [rt_b])
                    tt_(rt[:, 56:64], rt[:, 56:64], rt[:, 24:32], ALU.mult, [rt_b], [rt_b])
                    tt_(selacc, selacc, selb, ALU.add, [selacc_b, selb_b], [selacc_b])
                    P.emit("dve", lambda e, o=rt[:, 64:72], i=rt[:, 56:64]: e.max(out=o, in_=i), [rt_b], [rt_b])
                    ts_(IDX[:, ch, :], rt[:, 64:66], -1.0, None, ALU.add, None, [rt_b], [idx_b[ch]])
                    for kk in range(2):
                        ts_(rt[:, 72:80], rt[:, 56:64], rt[:, 64 + kk:65 + kk], None, ALU.is_equal, None, [rt_b], [rt_b])
                        tt_(rt[:, 72:80], rt[:, 72:80], rt[:, 48:56], ALU.mult, [rt_b], [rt_b])
                        P.emit("dve", lambda e, o=W12[:, ch, kk:kk + 1], i=rt[:, 72:80]: e.tensor_reduce(out=o, in_=i, axis=AX.X, op=ALU.add),
                               [rt_b], [w12_b[ch]])
                    pt_, ptb_ = psum("c")
                    ptv = pt_.bitcast(BF16)
                    for c in range(KC):
                        P.emit("pe", lambda e, o=ptv[:, c * 128:(c + 1) * 128], i=hT[:, c, csl]: e.transpose(o, i, ident_b),
                               [hT_b[c][tt], identb_b], [ptb_])
                    hk, hkb = htok[ch % 2], htok_b[ch % 2]
                    dcopy(hk, ptv[:, 0:D], [ptb_], [hkb])
                    for kk in range(2):
                        P.emit("pool", lambda e, o=hs_d[:, :], ix=IDX[:, ch, kk:kk + 1], i=hk: e.indirect_dma_start(
                            out=o, out_offset=bass.IndirectOffsetOnAxis(ap=ix, axis=0), in_=i, in_offset=None),
                            [hkb, idx_b[ch]], [hs_bs[ch]], dma_key="hs_sc")
                pc_, pcb_ = psum("b")
                mm(pc_[:, 0:8], ones_b16, selacc, True, True, [cb_b, selacc_b], [pcb_])
                dcopy(cnt_i, pc_[:, 0:8], [pcb_], [cnt_b])
                A.release()
                if dbg and sq_i == 0:
                    debug_dump("cnt", cnt_i, [128, 8], I32, [cnt_b])
                    debug_dump("idx", IDX, [128, NCH, 2], I32, flat(idx_b))
                    debug_dump("w12", W12, [128, NCH, 2], F32, flat(w12_b))

                P.barrier(); A.mark()
                acc = A.alloc((KC, 1024), F32); acc_b = bufs("acc", 3)
                hTe = A.alloc((KC, 1024), BF16); hTe_b = bufs("hTe", 3)
                w1s = [A.alloc((KC, 512), BF16) for _ in range(2)]; w1_b = bufs("w1s", 2)
                w3s = [A.alloc((KC, 512), BF16) for _ in range(2)]; w3_b = bufs("w3s", 2)
                w2s = [A.alloc((4, D), BF16) for _ in range(2)]; w2_b = bufs("w2s", 2)
                aT = [A.alloc((4, TT), BF16) for _ in range(2)]; aT_b = bufs("aT", 2, 4)
                sil = [A.alloc((TT,), F32) for _ in range(2)]; sil_b = bufs("sil", 2)
                NHS = 4
                hsin = [A.alloc((D,), BF16) for _ in range(NHS)]; hsin_b = bufs("hsin", NHS)
                ytoks = [A.alloc((D,), F32) for _ in range(2)]; ytoks_b = bufs("ytok", 2)
                yti = 0
                NFB = DFE // 512
                wsl = 0
                hsi = 0
                ai = [0]
                for st, ex in [(st_, ex_) for st_ in range(2) for ex_ in range(NEXP)]:
                    for eng_ in ENGS:
                        P.emit(eng_, lambda e, en=eng_, a=cnt_i[0:1, ex:ex + 1]: e.reg_load(P.REG[en], a), [cnt_b], ())
                    W1, W3, W2 = mw1_d[0, ex], mw3_d[0, ex], mw2_d[0, ex]
                    if st == 1:
                        P.cond_begin(1024)
                    if st == 0:
                        pieces = [(0, 0, 512, None), (1, 512, 256, 512), (1, 768, 256, 768)]
                    else:
                        pieces = [(2, 0, 512, None), (3, 512, 512, None)]

                    def grp(thr):
                        if thr is not None:
                            P.cond_begin(thr)

                    def endgrp(thr):
                        if thr is not None:
                            P.cond_end()

                    for pi, (k, pc0, pw, thr) in enumerate(pieces):
                        grp(thr)
                        for sb in range(pw // 128):
                            hi_, hib_ = hsin[hsi % NHS], hsin_b[hsi % NHS]; hsi += 1
                            c0_ = pc0 + sb * 128
                            r0_ = ex * CAP + st * 1024 + c0_
                            dma("sp", hi_, hs_d[r0_:r0_ + 128, :], hs_bs, [hib_], f"hsin{(hsi - 1) % NHS}")
                            pt_, ptb_ = psum("b")
                            ptv = pt_.bitcast(BF16)
                            for c in range(KC):
                                P.emit("pe", lambda e, o=ptv[:, c * 128:(c + 1) * 128], i=hi_[:, c * 128:(c + 1) * 128]: e.transpose(o, i, ident_b),
                                       [hib_, identb_b], [ptb_])
                            dcopy(hTe[:, :, c0_:c0_ + 128], ptv[:, 0:D].rearrange("p (c t) -> p c t", c=KC), [ptb_], [hTe_b[pi]])
                        endgrp(thr)
                    units = [(fb, pi) for fb in range(NFB) for pi in range(len(pieces))]
                    ust = {}
                    wbase = wsl
                    wsl += NFB

                    def up(fb, pi):
                        s_ = (wbase + fb) % 2
                        if pi == 0:
                            f0 = fb * 512
                            wload(w1s[s_], w1_b[s_], W1, f0, 512, KC, f"w1s{s_}")
                            wload(w3s[s_], w3_b[s_], W3, f0, 512, KC, f"w3s{s_}")
                            src = W2.rearrange("(c p) n -> p c n", p=128)[:, f0 // 128:f0 // 128 + 4, :]
                            dma("pool", w2s[s_], src, (), [w2_b[s_]], f"w2s{s_}")
                        k, pc0, pw, thr = pieces[pi]
                        grp(thr)
                        a_, a_b = aT[ai[0] % 2], aT_b[ai[0] % 2]; ai[0] += 1
                        ust[(fb, pi)] = (a_, a_b)
                        for j in range(4):
                            p1, p1b = psum("a")
                            for c in range(KC):
                                mm(p1[:, 0:pw], w1s[s_][:, c, j * 128:(j + 1) * 128], hTe[:, c, pc0:pc0 + pw], c == 0, c == KC - 1,
                                   [w1_b[s_], hTe_b[pi]], [p1b])
                            p3, p3b = psum("d")
                            for c in range(KC):
                                mm(p3[:, 0:pw], w3s[s_][:, c, j * 128:(j + 1) * 128], hTe[:, c, pc0:pc0 + pw], c == 0, c == KC - 1,
                                   [w3_b[s_], hTe_b[pi]], [p3b])
                            sl_, sl_b = sil[j % 2], sil_b[j % 2]
                            act(sl_[:, 0:pw], p1[:, 0:pw], AF.Silu, [p1b], [sl_b])
                            tt_(a_[:, j, 0:pw], sl_[:, 0:pw], p3[:, 0:pw], ALU.mult, [sl_b, p3b], [a_b[j]])
                        endgrp(thr)

                    def down(fb, pi):
                        s_ = (wbase + fb) % 2
                        k, pc0, pw, thr = pieces[pi]
                        a_, a_b = ust.pop((fb, pi))
                        grp(thr)
                        for c2 in range(KC):
                            po_, pob_ = psum("c")
                            for j in range(4):
                                mm(po_[:, 0:pw], w2s[s_][:, j, c2 * 128:(c2 + 1) * 128], a_[:, j, 0:pw], j == 0, j == 3, [w2_b[s_], a_b[j]], [pob_])
                            av = acc[:, c2, pc0:pc0 + pw]
                            if fb == 0:
                                dcopy(av, po_[:, 0:pw], [pob_], [acc_b[pi]])
                            else:
                                tt_(av, av, po_[:, 0:pw], ALU.add, [acc_b[pi], pob_], [acc_b[pi]])
                        endgrp(thr)

                    up(*units[0])
                    for ui in range(len(units)):
                        if ui + 1 < len(units):
                            up(*units[ui + 1])
                        down(*units[ui])
                    for pi, (k, pc0, pw, thr) in enumerate(pieces):
                        grp(thr)
                        for sb in range(pw // 128):
                            c0_ = pc0 + sb * 128
                            ytok, ytok_b = ytoks[yti % 2], ytoks_b[yti % 2]; yti += 1
                            for hf in range(2):
                                pt_, ptb_ = psum("b")
                                for c4 in range(4):
                                    c = hf * 4 + c4
                                    P.emit("pe", lambda e, o=pt_[:, c4 * 128:(c4 + 1) * 128], i=acc[:, c, c0_:c0_ + 128]: e.transpose(o, i, ident),
                                           [acc_b[pi], cf_b], [ptb_])
                                dcopy(ytok[:, hf * 512:(hf + 1) * 512], pt_, [ptb_], [ytok_b])
                            r0_ = ex * CAP + st * 1024 + c0_
                            dma("sp", ys_d[r0_:r0_ + 128, :], ytok, [ytok_b], [ys_bs[ex * 4 + k]], f"ys_wr{(yti - 1) % 2}")
                        endgrp(thr)
                    if st == 1:
                        P.cond_end()
                A.release()

                P.barrier(); A.mark()
                yg = [[A.alloc((D,), F32) for _ in range(2)] for _ in range(2)]; yg_b = bufs("yg", 2, 2)
                fo = [A.alloc((D,), F32) for _ in range(2)]; fo_b = bufs("fo", 2)
                for ch in range(NCH):
                    tt = ch // 4
                    csl = slice(ch * 128, (ch + 1) * 128)
                    r_ = ch % 2
                    for kk in range(2):
                        P.emit("pool", lambda e, o=yg[r_][kk], ix=IDX[:, ch, kk:kk + 1], i=ys_d[:, :]: e.indirect_dma_start(
                            out=o, out_offset=None, in_=i, in_offset=bass.IndirectOffsetOnAxis(ap=ix, axis=0)),
                            ys_bs + [idx_b[ch]], [yg_b[r_][kk]], dma_key=f"yg{r_}{kk}")
                    f_, f_b = fo[r_], fo_b[r_]
                    ts_(f_, yg[r_][0], W12[:, ch, 0:1], None, ALU.mult, None, [yg_b[r_][0], w12_b[ch]], [f_b])
                    stt_(f_, yg[r_][1], W12[:, ch, 1:2], f_, ALU.mult, ALU.add, [yg_b[r_][1], w12_b[ch], f_b], [f_b])
                    for hf in range(2):
                        pt_, ptb_ = psum("b")
                        for c4 in range(4):
                            c = hf * 4 + c4
                            P.emit("pe", lambda e, o=pt_[:, c4 * 128:(c4 + 1) * 128], i=f_[:, c * 128:(c + 1) * 128]: e.transpose(o, i, ident),
                                   [f_b, cf_b], [ptb_])
                        for c4 in range(4):
                            c = hf * 4 + c4
                            tt_(xT[:, c, csl], xT[:, c, csl], pt_[:, c4 * 128:(c4 + 1) * 128], ALU.add, [xT_b[c][tt], ptb_], [xT_b[c][tt]])
                A.release()
                A.release()
                if dbg and sq_i == 0:
                    debug_dump(f"xffn_{l}", xT, [128, KC, S], F32, flat(xT_b))
                continue
            hT = A.alloc((KC, S), BF16); hT_b = bufs("hT2", KC, NT)
            sqtmp = [A.alloc((TT,), F32) for _ in range(2)]; sq_b = bufs("sq", 2)
            rmsnorm_to_h(("nffn", l), hT, hT_b, sqtmp, sq_b)
            w1s = [A.alloc((KC, 512), BF16) for _ in range(2)]; w1_b = bufs("w1s", 2)
            w3s = [A.alloc((KC, 512), BF16) for _ in range(2)]; w3_b = bufs("w3s", 2)
            w2s = [A.alloc((4, D), BF16) for _ in range(2)]; w2_b = bufs("w2s", 2)
            aT = [A.alloc((4, TT), BF16) for _ in range(2)]; aT_b = bufs("aT", 2, 4)
            sil = [A.alloc((TT,), F32) for _ in range(2)]; sil_b = bufs("sil", 2)
            moe = (l == 1)
            if moe:
                cwb = A.alloc((S,), F32); cwb_b = bufs("cwb", NT)
                cwT = A.alloc((S,), F32); cwT_b = bufs("cwT", NT)
                wr = A.alloc((KC, NEXP), F32); wr_b = Buf("wr")
                rt = A.alloc((64,), F32); rt_b = Buf("rt")
                dma("sp", wr, router_d[0].rearrange("(c p) e -> p c e", p=128), (), [wr_b], "wr")
                for c in range(KC):
                    ts_(wr[:, c, :], wr[:, c, :], vcol(("nffn", l), c), None, ALU.mult, None, [wr_b, vecs_b], [wr_b])
                for ch in range(NCH):
                    tt = ch // 4
                    csl = slice(ch * 128, (ch + 1) * 128)
                    pl_, plb_ = psum("a")
                    for c in range(KC):
                        mm(pl_[:, 0:NEXP], xT[:, c, csl], wr[:, c, :], c == 0, c == KC - 1, [xT_b[c][tt], wr_b], [plb_])
                    mm(pl_[:, 16:17], rstd[:, csl], c256[:, 0:1], True, True, [rstd_b[tt], cf_b], [plb_])
                    ts_(rt[:, 16:17], pl_[:, 16:17], 2.0, None, ALU.mult, None, [plb_], [rt_b])
                    ts_(rt[:, 0:8], pl_[:, 0:8], rt[:, 16:17], None, ALU.mult, None, [plb_, rt_b], [rt_b])
                    P.emit("dve", lambda e, o=rt[:, 8:16], i=rt[:, 0:8]: e.max(out=o, in_=i), [rt_b], [rt_b])
                    ts_(rt[:, 17:18], rt[:, 8:9], -1.0, None, ALU.mult, None, [rt_b], [rt_b])
                    ts_(rt[:, 24:32], rt[:, 0:8], rt[:, 9:10], None, ALU.is_ge, None, [rt_b], [rt_b])
                    act(rt[:, 32:40], rt[:, 0:8], AF.Exp, [rt_b], [rt_b], bias=rt[:, 17:18])
                    tt_(rt[:, 32:40], rt[:, 32:40], rt[:, 24:32], ALU.mult, [rt_b], [rt_b])
                    P.emit("dve", lambda e, o=rt[:, 40:41], i=rt[:, 32:40]: e.tensor_reduce(out=o, in_=i, axis=AX.X, op=ALU.add), [rt_b], [rt_b])
                    P.emit("dve", lambda e, o=rt[:, 41:42], i=rt[:, 40:41]: e.reciprocal(out=o, in_=i), [rt_b], [rt_b])
                    ts_(rt[:, 48:56], rt[:, 32:40], rt[:, 41:42], None, ALU.mult, None, [rt_b], [rt_b])
                    ptr, ptrb = psum("b")
                    P.emit("pe", lambda e, o=ptr[0:8, 0:128], i=rt[:, 48:56]: e.transpose(o, i, ident), [rt_b, cf_b], [ptrb])
                    dcopy(cwT[0:8, csl], ptr[0:8, 0:128], [ptrb], [cwT_b[tt]])
                experts = [(mw1_d[0, e], mw3_d[0, e], mw2_d[0, e], DFE, e) for e in range(NEXP)]
            else:
                experts = [(dw1_d[0], dw3_d[0], dw2_d[0], DFF, None)]
            steps = []
            for (W1, W3, W2, dff, e) in experts:
                f0 = 0
                while f0 < dff:
                    n = min(512, dff - f0)
                    steps.append((W1, W3, W2, f0, n, e))
                    f0 += n

            def load_step(si):
                W1, W3, W2, f0, n, e = steps[si]
                s_ = si % 2
                wload(w1s[s_], w1_b[s_], W1, f0, n, KC, f"w1s{s_}")
                wload(w3s[s_], w3_b[s_], W3, f0, n, KC, f"w3s{s_}")
                src = W2.rearrange("(c p) n -> p c n", p=128)[:, f0 // 128:f0 // 128 + n // 128, :]
                dma("pool", w2s[s_][:, 0:n // 128, :], src, (), [w2_b[s_]], f"w2s{s_}")

            load_step(0)
            ai = 0
            cur_e = -1
            for si, (W1, W3, W2, f0, n, e) in enumerate(steps):
                if si + 1 < len(steps):
                    load_step(si + 1)
                s_ = si % 2
                nj = n // 128
                if moe and e != cur_e:
                    cur_e = e
                    for tt in range(NT):
                        sl = slice(tt * TT, (tt + 1) * TT)
                        pc_, pcb_ = psum("b")
                        mm(pc_, sel[0:8, e * 128:(e + 1) * 128], cwT[0:8, sl], True, True, [cf_b, cwT_b[tt]], [pcb_])
                        dcopy(cwb[:, sl], pc_, [pcb_], [cwb_b[tt]])
                for tt in range(NT):
                    sl = slice(tt * TT, (tt + 1) * TT)
                    a_, a_b = aT[ai % 2], aT_b[ai % 2]; ai += 1
                    for j in range(nj):
                        p1, p1b = psum("a")
                        proj_fm(p1, p1b, w1s[s_], w1_b[s_], j * 128, hT, hT_b, tt)
                        p3, p3b = psum("d")
                        proj_fm(p3, p3b, w3s[s_], w3_b[s_], j * 128, hT, hT_b, tt)
                        sl_, sl_b = sil[j % 2], sil_b[j % 2]
                        act(sl_, p1, AF.Silu, [p1b], [sl_b])
                        if moe:
                            tt_(sl_, sl_, cwb[:, sl], ALU.mult, [sl_b, cwb_b[tt]], [sl_b])
                        tt_(a_[:, j, :], sl_, p3, ALU.mult, [sl_b, p3b], [a_b[j]])
                    for c2 in range(KC):
                        po_, pob_ = psum("c")
                        for j in range(nj):
                            mm(po_, w2s[s_][:, j, c2 * 128:(c2 + 1) * 128], a_[:, j, :], j == 0, j == nj - 1, [w2_b[s_], a_b[j]], [pob_])
                        tt_(xT[:, c2, sl], xT[:, c2, sl], po_, ALU.add, [xT_b[c2][tt], pob_], [xT_b[c2][tt]])
            A.release()
            if dbg and sq_i == 0:
                debug_dump(f"xffn_{l}", xT, [128, KC, S], F32, flat(xT_b))
        else:
            P.barrier(); A.mark()
            sqtmp = [A.alloc((TT,), F32) for _ in range(2)]; sq_b = bufs("sq", 2)
            rmsnorm_to_h(None, None, None, sqtmp, sq_b)
            for tt in range(NT):
                sl = slice(tt * TT, (tt + 1) * TT)
                for c in range(KC):
                    stt_(xT[:, c, sl], xT[:, c, sl], vcol("nfinal", c), rstd[:, sl], ALU.mult, ALU.mult,
                         [xT_b[c][tt], rstd_b[tt], vecs_b], [xT_b[c][tt]])
            for c in range(KC):
                dma("sp", out_d[sq_i, c], xT[:, c, :], xT_b[c], (), f"out{c}")
            A.release()
            P.barrier()
            continue
        break

    P.finalize(nc, stack)
    stack.close()
    build_program.peak = A.peak
    return nc, list(dbg_out.keys())


def _fm(v):
    return np.ascontiguousarray(v.reshape(-1, 128).T)


def host_consts_and_layout(inp):
    f32 = np.float32
    vecs = np.zeros((128, NV), f32)
    for l in range(2):
        vecs[:, VL[("nmix", l)]:VL[("nmix", l)] + 8] = _fm(inp["norm_mix"][l])
        vecs[:, VL[("nffn", l)]:VL[("nffn", l)] + 8] = _fm(inp["norm_ffn"][l])
        vecs[:, VL[("bgate", l)]:VL[("bgate", l)] + 32] = _fm(inp["b_gate"][l])
        vecs[:, VL[("poolb", l)]:VL[("poolb", l)] + 2] = _fm(inp["pool_b"][l].reshape(-1))
        vecs[:, VL[("pools", l)]:VL[("pools", l)] + 2] = _fm(inp["pool_scale"][l])
        cw = inp["conv_w"][l]
        for j in range(2):
            vecs[:, VL[("convw", l)] + j * 31:VL[("convw", l)] + (j + 1) * 31] = cw[:, j * 128:(j + 1) * 128].T
        vecs[:, VL[("convb", l)]:VL[("convb", l)] + 2] = _fm(inp["conv_b"][l])
        vecs[:, VL[("clng", l)]:VL[("clng", l)] + 2] = _fm(inp["conv_ln_g"][l])
        vecs[:, VL[("clnb", l)]:VL[("clnb", l)] + 2] = _fm(inp["conv_ln_b"][l])
    vecs[:, VL["nfinal"]:VL["nfinal"] + 8] = _fm(inp["norm_final"])
    wins = np.array([[2, 4], [8, 16]], f32)
    for j in range(2):
        w = np.where(np.arange(128) < 64, wins[j, 0], wins[j, 1]).astype(f32)
        vecs[:, VL["pinvw"] + j] = 1.0 / w
        t = np.arange(16, dtype=f32)[None, :]
        vecs[:, VL["pinv16"] + j * 16:VL["pinv16"] + (j + 1) * 16] = 1.0 / np.minimum(t + 1.0, w[:, None])
    rows = np.zeros((128, 2 * NROW), f32)
    for l in range(2):
        r0 = l * NROW
        rows[:, r0:r0 + 256] = inp["gmlp_ln_g"][l][None, :]
        rows[:, r0 + 256:r0 + 512] = inp["gmlp_ln_b"][l][None, :]
        rows[:, r0 + 512:r0 + 1024] = inp["gmlp_b_s"][l].reshape(1, 512)
    cf = np.zeros((128, CFW), f32)
    cf[:, 0:128] = np.eye(128, dtype=f32)
    cf[:, 128:256] = 1.0
    cf[:, 256:384] = 1.0 / 256.0
    sidx = np.arange(128)
    cf[:, 384:512] = (sidx[:, None] <= sidx[None, :]).astype(f32)
    for e in range(8):
        cf[e, 512 + e * 128:512 + (e + 1) * 128] = 1.0
        cf[:, 1536 + e] = e * CAP + 1.0
    cb = np.zeros((128, 256), f32)
    cb[:, 0:128] = (sidx[:, None] < sidx[None, :]).astype(f32)
    cb[:, 128:256] = 1.0
    cb = cb.astype(ml_dtypes.bfloat16)
    slopes = 2.0 ** (-8.0 * np.arange(1, 9) / 8.0)
    o = np.arange(16)[None, :, None]; tl = np.arange(128)[None, None, :]; sl = np.arange(128)[:, None, None]
    delta = 128 * o + tl - sl
    mult = ((delta <= 128).astype(np.float64) + ((delta % 4 == 0) & (delta <= 512)) + ((delta % 16 == 0) & (delta <= 2048)))
    mult = np.where(delta >= 0, mult, 0.0)
    mtab = np.stack([mult * np.exp(-s * np.maximum(delta, 0)) for s in slopes]).reshape(8, 128, 2048)
    mtab = mtab.astype(ml_dtypes.bfloat16)
    wst = np.ascontiguousarray(np.transpose(inp["gmlp_w_s"], (0, 3, 1, 2)))
    common = {
        "vecs": vecs, "rows": rows, "cf32": cf, "cb16": cb, "mtab": mtab, "wst": wst,
        "w_in": inp["w_in"], "pool_w": inp["pool_w"],
        "w_br_a": inp["w_br_a"], "w_br_b": inp["w_br_b"], "w_br_c": inp["w_br_c"], "w_br_d": inp["w_br_d"],
        "w_gate": inp["w_gate"], "w_out": inp["w_out"],
        "dense_w1": inp["dense_w1"], "dense_w3": inp["dense_w3"], "dense_w2": inp["dense_w2"],
        "moe_router": inp["moe_router"], "moe_w1": inp["moe_w1"], "moe_w3": inp["moe_w3"], "moe_w2": inp["moe_w2"],
    }
    return common


def x_to_fm(xs):
    n = xs.shape[0]
    return np.ascontiguousarray(xs.reshape(n, S, KC, 128).transpose(0, 2, 3, 1))


def fm_to_x(o):
    n = o.shape[0]
    return np.ascontiguousarray(o.transpose(0, 3, 1, 2).reshape(n, S, D))


_CACHE = {}


def kernel(**inputs):
    inp = {k: np.asarray(v) for k, v in inputs.items()}
    x = inp["x"].astype(np.float32, copy=False)
    B = x.shape[0]
    nseq = B // NCORES
    common = host_consts_and_layout(inp)
    if "nc" not in _CACHE:
        _CACHE["nc"] = build_program(nseq)[0]
    nc = _CACHE["nc"]
    in_maps = []
    for c in range(NCORES):
        m = dict(common)
        m["xT"] = x_to_fm(x[c * nseq:(c + 1) * nseq])
        in_maps.append(m)
    res = run_bass_kernel_spmd(nc, in_maps, core_ids=list(range(NCORES)))
    outs = [fm_to_x(np.asarray(r["outT"])) for r in res.results]
    return np.concatenate(outs, axis=0).astype(np.float32)
```

```python
import numpy as np
import ml_dtypes
import concourse.bass as bass
import concourse.mybir as mybir
from concourse.bass_utils import run_bass_kernel_spmd

F32 = mybir.dt.float32
BF16 = mybir.dt.bfloat16
AF = mybir.ActivationFunctionType
ALU = mybir.AluOpType
AX = mybir.AxisListType

NCORES = 8
S = 2048
TT = 512
NT = S // TT
NCH = S // 128
D = 1024
KC = 8
DFF = 2816
DFE = 3584
NEXP = 8
INW = 2816
RMS_EPS = 1e-6
LN_EPS = 1e-5
ENGS = ["pe", "act", "dve", "pool", "sp"]

VL = {}
_o = 0
for _l in range(2):
    for _n, _w in (("nmix", 8), ("nffn", 8), ("bgate", 32), ("poolb", 2), ("pools", 2), ("convw", 62),
                   ("convb", 2), ("clng", 2), ("clnb", 2)):
        VL[(_n, _l)] = _o
        _o += _w
VL["nfinal"] = _o; _o += 8
VL["pinvw"] = _o; _o += 2
VL["pinv16"] = _o; _o += 32
NV = _o
NROW = 2048


class Buf:
    __slots__ = ("name", "w", "rd", "rd_dma")

    def __init__(self, name):
        self.name = name
        self.w = None
        self.rd = {}
        self.rd_dma = []


class Op:
    __slots__ = ("eng", "fn", "deps", "signal", "is_dma", "dma_key", "dma_cnt", "sig_k", "group")


class Prog:
    R = 8

    def __init__(self):
        self.ops = {e: [] for e in ENGS}
        self.dma_cnt = {}
        self.last_dma = {}
        self.pending = {e: [] for e in ENGS}
        self.cur_group = None
        self.ngroups = 0

    def cond_begin(self, thr):
        self.ngroups += 1
        self.cur_group = (self.ngroups, thr)

    def cond_end(self):
        self.cur_group = None

    def emit(self, eng, fn, reads=(), writes=(), dma_key=None):
        op = Op()
        op.eng = eng; op.fn = fn; op.signal = False
        op.is_dma = dma_key is not None
        op.dma_key = dma_key; op.dma_cnt = 0; op.sig_k = -1
        op.group = self.cur_group
        deps = {}

        def add(d, kind):
            if d is None:
                return
            if (not d.is_dma) and d.eng == eng:
                if kind not in ("raw", "waw", "war") or eng == "pe":
                    return
            deps[id(d)] = d

        for b in reads:
            add(b.w, "raw")
        for b in writes:
            add(b.w, "waw")
            for r in b.rd.values():
                add(r, "war")
            for r in b.rd_dma:
                add(r, "war")
        for d in self.pending[eng]:
            add(d, "bar")
        self.pending[eng] = []
        for b in reads:
            if op.is_dma:
                b.rd_dma.append(op)
            else:
                b.rd[eng] = op
        for b in writes:
            b.w = op
            b.rd = {}
            b.rd_dma = []
        for d in deps.values():
            d.signal = True
        op.deps = list(deps.values())
        if op.is_dma:
            c = self.dma_cnt.get(dma_key, 0) + 1
            self.dma_cnt[dma_key] = c
            op.dma_cnt = c
            self.last_dma[dma_key] = op
        self.ops[eng].append(op)
        return op

    def barrier(self):
        lst = []
        for e in ENGS:
            for op in reversed(self.ops[e]):
                if not op.is_dma:
                    lst.append(op)
                    break
        lst += list(self.last_dma.values())
        for e in ENGS:
            self.pending[e] = list(lst)

    def finalize(self, nc, stack):
        R = self.R
        sems = {}
        for e in ("pe", "act", "dve", "pool", "sp"):
            k = 0
            for op in self.ops[e]:
                if op.signal and not op.is_dma:
                    op.sig_k = k
                    k += 1
            if k > 0:
                sems[e] = [stack.enter_context(nc.semaphore(f"s_{e}_{i}")) for i in range(min(R, k))]
        dsem = {key: stack.enter_context(nc.semaphore(f"d_{i}")) for i, key in enumerate(self.dma_cnt)}
        block = stack.enter_context(nc.Block())
        prog = self

        ENG_OBJ = {"pe": nc.tensor, "act": nc.scalar, "dve": nc.vector, "pool": nc.gpsimd, "sp": nc.sync}
        use_groups = any(op.group is not None for e_ in ENGS for op in prog.ops[e_])
        REG = {}
        if use_groups:
            for e_ in ENGS:
                REG[e_] = stack.enter_context(ENG_OBJ[e_].register(f"ncnt_{e_}"))
        prog.REG = REG

        def run(eng, e):
            waited = {}
            waited_d = {}
            last_sig = [-1]

            def emit_op(op):
                for d in op.deps:
                    if d.is_dma:
                        if waited_d.get(d.dma_key, 0) >= d.dma_cnt:
                            continue
                        e.wait_ge(dsem[d.dma_key], 16 * d.dma_cnt)
                        waited_d[d.dma_key] = d.dma_cnt
                    else:
                        if waited.get(d.eng, -1) >= d.sig_k:
                            continue
                        e.wait_ge(sems[d.eng][d.sig_k % R], d.sig_k // R + 1)
                        waited[d.eng] = d.sig_k
                ins = op.fn(e)
                if op.is_dma:
                    ins.then_inc(dsem[op.dma_key], 16)
                elif op.signal:
                    ins.then_inc(sems[eng][op.sig_k % R], 1)
                    last_sig[0] = op.sig_k

            ops = prog.ops[eng]
            i = 0
            while i < len(ops):
                op = ops[i]
                if op.group is None:
                    emit_op(op)
                    i += 1
                    continue
                j = i
                while j < len(ops) and ops[j].group == op.group:
                    j += 1
                grp = ops[i:j]
                thr = op.group[1]
                sv_w, sv_d, sv_ls = dict(waited), dict(waited_d), last_sig[0]
                with e.If_lt(REG[eng], thr + 1):
                    if last_sig[0] >= 0:
                        k = last_sig[0]
                        e.wait_ge(sems[eng][k % R], k // R + 1)
                    cnts = {}
                    dk = {}
                    for g in grp:
                        if g.is_dma:
                            if g.dma_key not in dk:
                                dk[g.dma_key] = [g.dma_cnt - 1, 0]
                            dk[g.dma_key][1] += 1
                        elif g.signal:
                            cnts[g.sig_k % R] = cnts.get(g.sig_k % R, 0) + 1
                    for si_, c_ in cnts.items():
                        e.sem_inc(sems[eng][si_], c_)
                    for key, (before, n_) in dk.items():
                        if before > 0:
                            e.wait_ge(dsem[key], 16 * before)
                        e.sem_inc(dsem[key], 16 * n_)
                with e.Else():
                    for g in grp:
                        emit_op(g)
                waited.clear(); waited.update(sv_w)
                waited_d.clear(); waited_d.update(sv_d)
                i = j
            if eng == "sp":
                for key, c in prog.dma_cnt.items():
                    e.wait_ge(dsem[key], 16 * c)

        @block.tensor
        def _(e):
            run("pe", e)

        @block.scalar
        def _(e):
            run("act", e)

        @block.vector
        def _(e):
            run("dve", e)

        @block.gpsimd
        def _(e):
            run("pool", e)

        @block.sync
        def _(e):
            run("sp", e)


class Arena:
    def __init__(self, ap, n):
        self.ap = ap; self.n = n; self.off = 0; self.marks = []; self.peak = 0

    def alloc(self, free, dt):
        n = 1
        for f in free:
            n *= f
        nb = n * (2 if dt is F32 else 1)
        off = (self.off + 63) // 64 * 64
        assert off + nb <= self.n, f"arena overflow {off + nb} > {self.n}"
        v = self.ap[:, off:off + nb]
        if dt is F32:
            v = v.bitcast(F32)
        if len(free) == 2:
            v = v.rearrange("p (a b) -> p a b", a=free[0])
        elif len(free) == 3:
            v = v.rearrange("p (a b c) -> p a b c", a=free[0], b=free[1])
        self.off = off + nb
        self.peak = max(self.peak, self.off)
        return v

    def mark(self):
        self.marks.append(self.off)

    def release(self):
        self.off = self.marks.pop()


def bufs(name, *dims):
    if len(dims) == 0:
        return Buf(name)
    return [bufs(f"{name}_{i}", *dims[1:]) for i in range(dims[0])]


ARENA_N = 105600
CAP = 2048
CFW = 128 * 4 + 1024 + 8
ROUTED = True


def build_program(nseq=2, stop_after=None, dbg=False):
    nc = bass.Bass("TRN2", target_bir_lowering=False)
    from contextlib import ExitStack
    stack = ExitStack()
    P = Prog()

    def dram(name, shape, dt=F32, kind="ExternalInput"):
        return nc.dram_tensor(name, list(shape), dt, kind=kind).ap()

    x_d = dram("xT", [nseq, KC, 128, S])
    out_d = dram("outT", [nseq, KC, 128, S], kind="ExternalOutput")
    vecs_d = dram("vecs", [128, NV])
    rows_d = dram("rows", [128, 2 * NROW])
    cf_d = dram("cf32", [128, CFW])
    cb_d = dram("cb16", [128, 256], BF16)
    hs_d = dram("hs_scratch", [NEXP * CAP, D], BF16, kind="Internal")
    ys_d = dram("ys_scratch", [NEXP * CAP, D], F32, kind="Internal")
    mtab_d = dram("mtab", [8, 128, 2048], BF16)
    w_in_d = dram("w_in", [2, D, INW])
    wst_d = dram("wst", [2, 128, 4, 128])
    pool_w_d = dram("pool_w", [2, 4, 64, 64])
    wbr_d = [dram("w_br_a", [2, 256, D]), dram("w_br_b", [2, 256, D]), dram("w_br_c", [2, 512, D]), dram("w_br_d", [2, 256, D])]
    w_gate_d = dram("w_gate", [2, D, 4 * D])
    w_out_d = dram("w_out", [2, D, D])
    dw1_d = dram("dense_w1", [1, D, DFF]); dw3_d = dram("dense_w3", [1, D, DFF]); dw2_d = dram("dense_w2", [1, DFF, D])
    router_d = dram("moe_router", [1, D, NEXP])
    mw1_d = dram("moe_w1", [1, NEXP, D, DFE]); mw3_d = dram("moe_w3", [1, NEXP, D, DFE]); mw2_d = dram("moe_w2", [1, NEXP, DFE, D])
    dbg_out = {}

    arena_t = stack.enter_context(nc.sbuf_tensor("arena", [128, ARENA_N], BF16))
    A = Arena(arena_t, ARENA_N)
    psb = [stack.enter_context(nc.psum_tensor(f"ps{i}", [128, 512], F32)) for i in range(8)]
    PS = [(psb[i][:, :], Buf(f"ps{i}")) for i in range(8)]
    rings = {"a": [0, 1], "b": [2, 3], "c": [4, 5], "d": [6, 7], "s": [0, 1, 2, 3, 6, 7]}
    rpos = {k: 0 for k in rings}

    def psum(ring):
        lst = rings[ring]
        i = lst[rpos[ring] % len(lst)]
        rpos[ring] += 1
        return PS[i]

    def mm(out, lhsT, rhs, start, stop, reads, writes):
        P.emit("pe", lambda e: e.matmul(out, lhsT, rhs, start=start, stop=stop), reads, writes)

    def act(out, in_, func, reads, writes, bias=None, scale=None):
        kw = {}
        if bias is not None:
            kw["bias"] = bias
        if scale is not None:
            kw["scale"] = scale
        P.emit("act", lambda e: e.activation(out=out, in_=in_, func=func, **kw), reads, writes)

    def tt_(out, in0, in1, op, reads, writes):
        P.emit("dve", lambda e: e.tensor_tensor(out=out, in0=in0, in1=in1, op=op), reads, writes)

    def ts_(out, in0, s1, s2, op0, op1, reads, writes):
        if op1 is None:
            P.emit("dve", lambda e: e.tensor_scalar(out=out, in0=in0, scalar1=s1, scalar2=None, op0=op0), reads, writes)
        else:
            P.emit("dve", lambda e: e.tensor_scalar(out=out, in0=in0, scalar1=s1, scalar2=s2, op0=op0, op1=op1), reads, writes)

    def stt_(out, in0, scalar, in1, op0, op1, reads, writes):
        P.emit("dve", lambda e: e.scalar_tensor_tensor(out=out, in0=in0, scalar=scalar, in1=in1, op0=op0, op1=op1), reads, writes)

    def dcopy(out, in_, reads, writes):
        P.emit("dve", lambda e: e.tensor_copy(out=out, in_=in_), reads, writes)

    def dma(q, out, in_, reads, writes, key):
        P.emit(q, lambda e: e.dma_start(out=out, in_=in_), reads, writes, dma_key=key)

    def memset(ap, val, writes):
        P.emit("dve", lambda e: e.memset(ap, val), (), writes)

    def debug_dump(name, view, shape, dt, rbufs):
        if not dbg:
            return
        d = dram("dbg_" + name, shape, dt, kind="ExternalOutput")
        dbg_out[name] = 1
        dma("sp", d, view, rbufs, (), "dbg_" + name)

    xT = A.alloc((KC, S), F32); xT_b = bufs("xT", KC, NT)
    cf = A.alloc((CFW,), F32); cf_b = Buf("cf")
    offs = cf[:, 1536:1544]
    cb16 = A.alloc((256,), BF16); cb_b = Buf("cb16")
    ustrict = cb16[:, 0:128]; ones_b16 = cb16[:, 128:256]
    ident_b = A.alloc((128,), BF16); identb_b = Buf("identb")
    hs_bs = bufs("hs", NCH); ys_bs = bufs("ys", NEXP * 4)
    ident = cf[:, 0:128]; ones_f = cf[:, 128:256]; c256 = cf[:, 256:384]; cmask = cf[:, 384:512]; sel = cf[:, 512:1536]
    vecs = A.alloc((NV,), F32); vecs_b = Buf("vecs")
    rows_b = Buf("rows")
    rstd = A.alloc((S,), F32); rstd_b = bufs("rstd", NT)
    dma("sp", cf, cf_d, (), [cf_b], "cf")
    dma("sp", cb16, cb_d, (), [cb_b], "cb16")
    dcopy(ident_b, ident, [cf_b], [identb_b])
    dma("sp", vecs, vecs_d, (), [vecs_b], "vecs")

    def vcol(key, j=0, n=1):
        o = VL[key] + j
        return vecs[:, o:o + n]

    def flat(*ls):
        r = []
        for l in ls:
            if isinstance(l, Buf):
                r.append(l)
            else:
                r.extend(flat(*l))
        return r

    def rmsnorm_to_h(gkey, hT, hT_b, sqtmp, sq_b):
        for tt in range(NT):
            sl = slice(tt * TT, (tt + 1) * TT)
            ps, pb = psum("a")
            for c in range(KC):
                sq, sqb = sqtmp[c % 2], sq_b[c % 2]
                act(sq, xT[:, c, sl], AF.Square, [xT_b[c][tt]], [sqb])
                mm(ps, ones_f, sq, c == 0, c == KC - 1, [sqb, cf_b], [pb])
            act(rstd[:, sl], ps, AF.Sqrt, [pb, eps_b], [rstd_b[tt]], bias=eps_rms, scale=1.0 / D)
            P.emit("dve", lambda e, o=rstd[:, sl]: e.reciprocal(out=o, in_=o), [rstd_b[tt]], [rstd_b[tt]])
            if hT is not None:
                for c in range(KC):
                    stt_(hT[:, c, sl], xT[:, c, sl], vcol(gkey, c), rstd[:, sl], ALU.mult, ALU.mult,
                         [xT_b[c][tt], rstd_b[tt], vecs_b], [hT_b[c][tt]])

    eps_t = A.alloc((4,), F32); eps_b = Buf("eps")
    memset(eps_t[:, 0:1], RMS_EPS, [eps_b])
    memset(eps_t[:, 1:2], LN_EPS, [eps_b])
    eps_rms = eps_t[:, 0:1]; eps_ln = eps_t[:, 1:2]

    def wload(slot, slot_b, w2d, c0, n, kchunks, key, k0=0):
        src = w2d.rearrange("(c p) n -> p c n", p=128)[:, k0:k0 + kchunks, c0:c0 + n]
        dma("pool", slot[:, 0:kchunks, 0:n], src, (), [slot_b], key)

    def proj_fm(ps, pb, w, wb, col, hT, hT_b, tt, kchunks=KC, koff=0, width=TT, t0=None):
        if t0 is None:
            t0 = tt * TT
        for k in range(kchunks):
            mm(ps[:, 0:width], w[:, k, col:col + 128], hT[:, koff + k, t0:t0 + width], k == 0, k == kchunks - 1,
               [wb, hT_b[koff + k][tt]], [pb])

    for sq_i in range(nseq):
        for c in range(KC):
            dma("sp", xT[:, c, :], x_d[sq_i, c], (), xT_b[c], f"x{c}")
        for l in range(2):
            P.barrier()
            A.mark()
            hT = A.alloc((KC, S), BF16); hT_b = bufs("hT", KC, NT)
            yT = A.alloc((10, S), BF16); yT_b = bufs("yT", 10, NT)
            A.mark()
            sqtmp = [A.alloc((TT,), F32) for _ in range(2)]; sq_b = bufs("sq", 2)
            rmsnorm_to_h(("nmix", l), hT, hT_b, sqtmp, sq_b)
            A.release()
            if dbg and sq_i == 0:
                debug_dump(f"h_{l}", hT, [128, KC, S], BF16, flat(hT_b))
            w_in_l = w_in_d[l]

            P.barrier(); A.mark()
            ws = A.alloc((KC, 512), BF16); ws_b = Buf("ws_d")
            wload(ws, ws_b, w_in_l, 2304, 512, KC, "ws_d")
            ypad = [A.alloc((2, 30 + TT), BF16) for _ in range(2)]; ypad_b = bufs("ypad", 2, 2)
            cacc = A.alloc((2, TT), F32); cacc_b = bufs("cacc", 2)
            sg = [A.alloc((TT,), F32) for _ in range(2)]; sg_b = bufs("sg", 2)
            lnt = [A.alloc((TT,), F32) for _ in range(3)]; lnt_b = bufs("lnt", 3)
            dg = A.alloc((62, 128), BF16); dg_b = Buf("dg")
            for j in range(2):
                cw0 = VL[("convw", l)] + j * 31
                for k in range(31):
                    ts_(dg[:, j * 31 + k, :], ident, vecs[:, cw0 + k:cw0 + k + 1], None, ALU.mult, None, [cf_b, vecs_b], [dg_b])
            for i in range(2):
                for j in range(2):
                    memset(ypad[i][:, j, 0:30], 0.0, [ypad_b[i][j]])
            def d_front(tt):
                yp, ypb = ypad[tt % 2], ypad_b[tt % 2]
                if tt > 0:
                    ypp, yppb = ypad[(tt - 1) % 2], ypad_b[(tt - 1) % 2]
                    for j in range(2):
                        dcopy(yp[:, j, 0:30], ypp[:, j, TT:TT + 30], [yppb[j]], [ypb[j]])
                for j in range(2):
                    pa, pab = psum("a")
                    proj_fm(pa, pab, ws, ws_b, j * 128, hT, hT_b, tt)
                    pg, pgb = psum("b")
                    proj_fm(pg, pgb, ws, ws_b, 256 + j * 128, hT, hT_b, tt)
                    act(sg[j], pg, AF.Sigmoid, [pgb], [sg_b[j]])
                    tt_(yp[:, j, 30:30 + TT], pa, sg[j], ALU.mult, [pab, sg_b[j]], [ypb[j]])

            def d_back(tt):
                sl = slice(tt * TT, (tt + 1) * TT)
                yp, ypb = ypad[tt % 2], ypad_b[tt % 2]
                for j in range(2):
                    pc, pcb = psum("c" if j == 0 else "d")
                    for k in range(31):
                        mm(pc, dg[:, j * 31 + k, :], yp[:, j, k:k + TT], k == 0, k == 30, [dg_b, ypb[j]], [pcb])
                    act(cacc[:, j, :], pc, AF.Identity, [pcb, vecs_b], [cacc_b[j]], bias=vcol(("convb", l), j))
                mean, var, rs = lnt[0], lnt[1], lnt[2]
                pm, pmb = psum("c")
                for j in range(2):
                    mm(pm, c256, cacc[:, j, :], j == 0, j == 1, [cacc_b[j], cf_b], [pmb])
                pq, pqb = psum("d")
                for j in range(2):
                    act(var, cacc[:, j, :], AF.Square, [cacc_b[j]], [lnt_b[1]])
                    mm(pq, c256, var, j == 0, j == 1, [lnt_b[1], cf_b], [pqb])
                dcopy(mean, pm, [pmb], [lnt_b[0]])
                tt_(var, mean, mean, ALU.mult, [lnt_b[0]], [lnt_b[1]])
                tt_(var, pq, var, ALU.subtract, [pqb, lnt_b[1]], [lnt_b[1]])
                act(rs, var, AF.Sqrt, [lnt_b[1], eps_b], [lnt_b[2]], bias=eps_ln)
                P.emit("dve", lambda e, o=rs: e.reciprocal(out=o, in_=o), [lnt_b[2]], [lnt_b[2]])
                for j in range(2):
                    tt_(cacc[:, j, :], cacc[:, j, :], mean, ALU.subtract, [cacc_b[j], lnt_b[0]], [cacc_b[j]])
                    tt_(cacc[:, j, :], cacc[:, j, :], rs, ALU.mult, [cacc_b[j], lnt_b[2]], [cacc_b[j]])
                    act(yT[:, 8 + j, sl], cacc[:, j, :], AF.Silu, [cacc_b[j], vecs_b], [yT_b[8 + j][tt]],
                        bias=vcol(("clnb", l), j), scale=vcol(("clng", l), j))

            d_front(0)
            for tt in range(NT):
                if tt + 1 < NT:
                    d_front(tt + 1)
                d_back(tt)
            A.release()

            P.barrier(); A.mark()
            ws = A.alloc((KC, 256), BF16); ws_b = Buf("ws_b")
            wload(ws, ws_b, w_in_l, 512, 256, KC, "ws_b")
            pwd = A.alloc((2, 128), BF16); pwd_b = Buf("pwd")
            memset(pwd, 0.0, [pwd_b])
            for g in range(4):
                j, hp = g // 2, g % 2
                dma("pool", pwd[hp * 64:(hp + 1) * 64, j, hp * 64:(hp + 1) * 64], pool_w_d[l, g], (), [pwd_b], "pwd")
            zp = [A.alloc((2, 16 + TT), F32) for _ in range(2)]; zp_b = bufs("zp", 2, 2)
            sA = A.alloc((16 + TT,), F32); sB = A.alloc((16 + TT,), F32); sAB_b = bufs("sAB", 2)
            pl = A.alloc((TT,), F32); pl_b = Buf("pl")
            plb = A.alloc((TT,), BF16); plb_b = Buf("plb")
            for i in range(2):
                for j in range(2):
                    memset(zp[i][:, j, 0:16], 0.0, [zp_b[i][j]])
            def b_front(tt):
                z, zb = zp[tt % 2], zp_b[tt % 2]
                if tt > 0:
                    zpp, zppb = zp[(tt - 1) % 2], zp_b[(tt - 1) % 2]
                    for j in range(2):
                        dcopy(z[:, j, 0:16], zpp[:, j, TT:TT + 16], [zppb[j]], [zb[j]])
                for j in range(2):
                    pz, pzb = psum("a")
                    proj_fm(pz, pzb, ws, ws_b, j * 128, hT, hT_b, tt)
                    dcopy(z[:, j, 16:16 + TT], pz, [pzb], [zb[j]])

            def b_back(tt):
                sl = slice(tt * TT, (tt + 1) * TT)
                z, zb = zp[tt % 2], zp_b[tt % 2]
                for j in range(2):
                    W = 16 + TT
                    tt_(sA[:, 1:W], z[:, j, 1:W], z[:, j, 0:W - 1], ALU.add, [zb[j]], [sAB_b[0]])
                    if j == 0:
                        tt_(sB[:, 3:W], sA[:, 3:W], sA[:, 1:W - 2], ALU.add, [sAB_b[0]], [sAB_b[1]])
                        lo, hi = sA, sB
                        lob, hib = sAB_b[0], sAB_b[1]
                    else:
                        tt_(sB[:, 3:W], sA[:, 3:W], sA[:, 1:W - 2], ALU.add, [sAB_b[0]], [sAB_b[1]])
                        tt_(sA[:, 7:W], sB[:, 7:W], sB[:, 3:W - 4], ALU.add, [sAB_b[1]], [sAB_b[0]])
                        tt_(sB[:, 15:W], sA[:, 15:W], sA[:, 7:W - 8], ALU.add, [sAB_b[0]], [sAB_b[1]])
                        lo, hi = sA, sB
                        lob, hib = sAB_b[0], sAB_b[1]
                    ivw = vecs[:, VL["pinvw"] + j:VL["pinvw"] + j + 1]
                    for (src, srcb, p0) in ((lo, lob, 0), (hi, hib, 64)):
                        ps_ = slice(p0, p0 + 64)
                        stt_(pl[ps_, :], src[ps_, 16:W], ivw[ps_, :], z[ps_, j, 16:W], ALU.mult, ALU.subtract,
                             [srcb, zb[j], vecs_b], [pl_b])
                    if tt == 0:
                        o16 = VL["pinv16"] + j * 16
                        for (src, srcb, p0) in ((lo, lob, 0), (hi, hib, 64)):
                            ps_ = slice(p0, p0 + 64)
                            tt_(pl[ps_, 0:16], src[ps_, 16:32], vecs[ps_, o16:o16 + 16], ALU.mult, [srcb, vecs_b], [pl_b])
                            tt_(pl[ps_, 0:16], pl[ps_, 0:16], z[ps_, j, 16:32], ALU.subtract, [pl_b, zb[j]], [pl_b])
                    dcopy(plb, pl, [pl_b], [plb_b])
                    po, pob = psum("b")
                    mm(po, pwd[:, j, :], plb, True, True, [pwd_b, plb_b], [pob])
                    ts_(yT[:, 2 + j, sl], po, vcol(("poolb", l), j), vcol(("pools", l), j), ALU.add, ALU.mult,
                        [pob, vecs_b], [yT_b[2 + j][tt]])

            b_front(0)
            for tt in range(NT):
                if tt + 1 < NT:
                    b_front(tt + 1)
                b_back(tt)
            A.release()

            P.barrier(); A.mark()
            rows = A.alloc((NROW,), F32)
            dma("sp", rows, rows_d[:, l * NROW:(l + 1) * NROW], (), [rows_b], "rows")
            ws = A.alloc((KC, 512), BF16); ws_b = Buf("ws_a")
            wload(ws, ws_b, w_in_l, 0, 512, KC, "ws_a")
            wsf = A.alloc((4, 128), F32); wsf_b = Buf("wsf")
            wsT = A.alloc((4, 128), BF16); wsT_b = Buf("wsT")
            dma("sp", wsf, wst_d[l], (), [wsf_b], "wsf")
            for g in range(4):
                tt_(wsT[:, g, :], wsf[:, g, :], cmask, ALU.mult, [wsf_b, cf_b], [wsT_b])
            uT = [A.alloc((2, TT), F32) for _ in range(2)]; uT_b = bufs("uT", 2, 2)
            vg = [A.alloc((256,), F32) for _ in range(2)]; vg_b = bufs("vg", 2)
            vln = [A.alloc((4, 256), BF16) for _ in range(2)]; vln_b = bufs("vln", 2, 4)
            st6 = A.alloc((8,), F32); st_b = Buf("st6")
            mv = A.alloc((4,), F32); mv_b = Buf("mv")
            ytmp = [A.alloc((128,), F32) for _ in range(2)]; ytmp_b = bufs("ytmp", 2)
            r0 = 0
            lng = rows[:, r0:r0 + 256]; lnb = rows[:, r0 + 256:r0 + 512]
            def a_front(tt):
                u, ub = uT[tt % 2], uT_b[tt % 2]
                vl, vlb = vln[tt % 2], vln_b[tt % 2]
                for j in range(2):
                    pu, pub = psum("a")
                    proj_fm(pu, pub, ws, ws_b, j * 128, hT, hT_b, tt)
                    act(u[:, j, :], pu, AF.Gelu_apprx_tanh, [pub], [ub[j]])
                for ci in range(4):
                    ch = tt * 4 + ci
                    pv, pvb = psum("b")
                    for k in range(KC):
                        mm(pv[:, 0:256], hT[:, k, ch * 128:(ch + 1) * 128], ws[:, k, 256:512], k == 0, k == KC - 1,
                           [hT_b[k][tt], ws_b], [pvb])
                    v_, v_b = vg[ci % 2], vg_b[ci % 2]
                    act(v_, pv[:, 0:256], AF.Gelu_apprx_tanh, [pvb], [v_b])
                    P.emit("dve", lambda e, o=st6[:, 0:6], i=v_: e.bn_stats(out=o, in_=i), [v_b], [st_b])
                    P.emit("dve", lambda e, o=mv[:, 0:2], i=st6[:, 0:6]: e.bn_aggr(out=o, in_=i), [st_b], [mv_b])
                    act(mv[:, 2:3], mv[:, 1:2], AF.Sqrt, [mv_b, eps_b], [mv_b], bias=eps_ln)
                    P.emit("dve", lambda e, o=mv[:, 3:4], i=mv[:, 2:3]: e.reciprocal(out=o, in_=i), [mv_b], [mv_b])
                    ts_(v_, v_, mv[:, 0:1], mv[:, 3:4], ALU.subtract, ALU.mult, [v_b, mv_b], [v_b])
                    tt_(v_, v_, lng, ALU.mult, [v_b, rows_b], [v_b])
                    tt_(vl[:, ci, :], v_, lnb, ALU.add, [v_b, rows_b], [vlb[ci]])

            def a_back(tt):
                u, ub = uT[tt % 2], uT_b[tt % 2]
                vl, vlb = vln[tt % 2], vln_b[tt % 2]
                for j in range(2):
                    for gl in range(2):
                        g = 2 * j + gl
                        pm_, pmb_ = psum("c")
                        for ci in range(4):
                            mm(pm_[:, ci * 128:(ci + 1) * 128], vl[:, ci, j * 128:(j + 1) * 128], wsT[:, g, :], True, True,
                               [vlb[ci], wsT_b], [pmb_])
                        ps_ = slice(gl * 64, gl * 64 + 64)
                        bs = rows[:, r0 + 512 + g * 128:r0 + 512 + (g + 1) * 128]
                        for ci in range(4):
                            yt, ytb = ytmp[ci % 2], ytmp_b[ci % 2]
                            tt_(yt[ps_, :], pm_[ps_, ci * 128:(ci + 1) * 128], bs[ps_, :], ALU.add, [pmb_, rows_b], [ytb])
                            t0 = tt * TT + ci * 128
                            tt_(yT[ps_, j, t0:t0 + 128], yt[ps_, :], u[ps_, j, ci * 128:(ci + 1) * 128], ALU.mult,
                                [ytb, ub[j]], [yT_b[j][tt]])

            a_front(0)
            for tt in range(NT):
                if tt + 1 < NT:
                    a_front(tt + 1)
                a_back(tt)
            A.release()

            P.barrier(); A.mark()
            wqs = [A.alloc((KC, 384), BF16) for _ in range(2)]; wqs_b = bufs("wq", 2, 3)

            def load_wq(jp_):
                for wi, c0 in enumerate((768, 1280, 1792)):
                    src = w_in_l.rearrange("(c p) n -> p c n", p=128)[:, :, c0 + jp_ * 128:c0 + (jp_ + 1) * 128]
                    dma("pool", wqs[jp_ % 2][:, :, wi * 128:(wi + 1) * 128], src, (), [wqs_b[jp_ % 2][wi]], f"wq{jp_ % 2}{wi}")

            load_wq(0)
            qT = A.alloc((S,), BF16); qT_b = bufs("qT", NT)
            kT = A.alloc((S,), BF16); kT_b = bufs("kT", NT)
            vaug = A.alloc((NCH, 2, 128), BF16); vaug_b = bufs("vaug", NT)
            NMT = 2
            NPT = 6
            mt = [A.alloc((2048,), BF16) for _ in range(NMT)]; mt_b = bufs("mt", NMT)
            pts = [A.alloc((TT,), BF16) for _ in range(NPT)]; pts_b = bufs("pts", NPT)
            rec = [A.alloc((TT,), F32) for _ in range(2)]; rec_b = bufs("rec", 2)
            memset(vaug[:, :, :, 64:128], 1.0, flat(vaug_b))
            pti = [0]
            for jp in range(4):
                wq, wq_b = wqs[jp % 2], wqs_b[jp % 2]
                if jp + 1 < 4:
                    load_wq(jp + 1)
                for tt in range(NT):
                    sl = slice(tt * TT, (tt + 1) * TT)
                    pq_, pqb_ = psum("a")
                    for k in range(KC):
                        mm(pq_, wq[:, k, 0:128], hT[:, k, sl], k == 0, k == KC - 1, [wq_b[0], hT_b[k][tt]], [pqb_])
                    act(qT[:, sl], pq_, AF.Copy, [pqb_], [qT_b[tt]], scale=0.125)
                    pk_, pkb_ = psum("a")
                    for k in range(KC):
                        mm(pk_, wq[:, k, 128:256], hT[:, k, sl], k == 0, k == KC - 1, [wq_b[1], hT_b[k][tt]], [pkb_])
                    dcopy(kT[:, sl], pk_, [pkb_], [kT_b[tt]])
                    pv_, pvb_ = psum("b")
                    for ci in range(4):
                        ch = tt * 4 + ci
                        for k in range(KC):
                            mm(pv_[:, ci * 128:(ci + 1) * 128], hT[:, k, ch * 128:(ch + 1) * 128], wq[:, k, 256:384],
                               k == 0, k == KC - 1, [wq_b[2], hT_b[k][tt]], [pvb_])
                    for ci in range(4):
                        ch = tt * 4 + ci
                        for hh in range(2):
                            dcopy(vaug[:, ch, hh, 0:64], pv_[:, ci * 128 + hh * 64:ci * 128 + hh * 64 + 64], [pvb_], [vaug_b[tt]])
                steps = []
                for hh in range(2):
                    for tt in range(NT):
                        for J in range(4 * tt + 4):
                            steps.append((hh, tt, J, 4 * tt + 4))
                LA = 3
                st_state = {}
                po_state = {}

                def front(i):
                    hh, tt, J, nJ = steps[i]
                    h = jp * 2 + hh
                    hb = hh * 64
                    t0 = tt * TT
                    m_, m_b = mt[h % NMT], mt_b[h % NMT]
                    if tt == 0 and J == 0:
                        dma("sp", m_, mtab_d[h], (), [m_b], f"mt{h % NMT}")
                    col_lo = max(0, J * 128 - t0)
                    wd = TT - col_lo
                    ps_, psb_ = psum("s")
                    mm(ps_[:, 0:wd], kT[hb:hb + 64, J * 128:(J + 1) * 128], qT[hb:hb + 64, t0 + col_lo:t0 + TT], True, True,
                       [kT_b[J // 4], qT_b[tt]], [psb_])
                    pt, ptb = pts[pti[0] % NPT], pts_b[pti[0] % NPT]
                    pti[0] += 1
                    act(pt[:, 0:wd], ps_[:, 0:wd], AF.Exp, [psb_], [ptb])
                    o_first = max(4 * tt, J) - J
                    tt_(pt[:, 0:wd], pt[:, 0:wd], m_[:, o_first * 128:o_first * 128 + wd], ALU.mult, [ptb, m_b], [ptb])
                    st_state[i] = (pt, ptb, col_lo, wd)

                def back(i):
                    hh, tt, J, nJ = steps[i]
                    hb = hh * 64
                    t0 = tt * TT
                    pt, ptb, col_lo, wd = st_state.pop(i)
                    if J == 0:
                        po_state[(hh, tt)] = psum("c")
                    po_, pob_ = po_state[(hh, tt)]
                    mm(po_[:, col_lo:TT], vaug[:, J, hh, :], pt[:, 0:wd], J == 0, J == nJ - 1, [vaug_b[J // 4], ptb], [pob_])
                    if J == nJ - 1:
                        rc, rcb = rec[tt % 2], rec_b[tt % 2]
                        act(rc[0:64, :], po_[64:128, :], AF.Ln, [pob_], [rcb])
                        act(rc[0:64, :], rc[0:64, :], AF.Exp, [rcb], [rcb], scale=-1.0)
                        tt_(yT[hb:hb + 64, 4 + jp, t0:t0 + TT], po_[0:64, :], rc[0:64, :], ALU.mult, [pob_, rcb], [yT_b[4 + jp][tt]])

                for i in range(len(steps) + LA):
                    if i < len(steps):
                        front(i)
                    if i - LA >= 0:
                        back(i - LA)
            A.release()
            if dbg and sq_i == 0:
                debug_dump(f"y_{l}", yT, [128, 10, S], BF16, flat(yT_b))
            if stop_after == ("mixers", l):
                break

            P.barrier(); A.mark()
            wg = [A.alloc((KC, 256), BF16) for _ in range(4)]; wg_b = bufs("wg", 4)
            wbr = A.alloc((10, 256), BF16); wbr_b = bufs("wbr", 4)
            wo = A.alloc((2, D), BF16); wo_b = Buf("wo")
            mg = A.alloc((2, S), BF16); mg_b = bufs("mg", 2, NT)
            gt = [A.alloc((TT,), F32) for _ in range(2)]; gt_b = bufs("gt", 2)
            acc = [A.alloc((TT,), F32) for _ in range(2)]; acc_b = bufs("acc", 2)
            tmpm = [A.alloc((TT,), F32) for _ in range(2)]; tmpm_b = bufs("tmpm", 2)
            koffs = (0, 2, 4, 8); kcs = (2, 2, 4, 2)
            gti = 0; aci = 0; tmi = 0
            for fg in range(4):
                for i in range(4):
                    wload(wg[i], wg_b[i], w_gate_d[l], i * D + fg * 256, 256, KC, f"wg{i}")
                    src = wbr_d[i][l].rearrange("(c p) n -> p c n", p=128)[:, :, fg * 256:(fg + 1) * 256]
                    dma("pool", wbr[:, koffs[i]:koffs[i] + kcs[i], :], src, (), [wbr_b[i]], f"wbr{i}")
                src = w_out_d[l].rearrange("(c p) n -> p c n", p=128)[:, fg * 2:fg * 2 + 2, :]
                dma("pool", wo, src, (), [wo_b], "wo")
                for tt in range(NT):
                    sl = slice(tt * TT, (tt + 1) * TT)
                    for cc in range(2):
                        c = fg * 2 + cc
                        ac, acb = acc[aci % 2], acc_b[aci % 2]; aci += 1
                        for i in range(4):
                            pg_, pgb_ = psum("a")
                            proj_fm(pg_, pgb_, wg[i], wg_b[i], cc * 128, hT, hT_b, tt)
                            g_, g_b = gt[gti % 2], gt_b[gti % 2]; gti += 1
                            act(g_, pg_, AF.Sigmoid, [pgb_, vecs_b], [g_b], bias=vcol(("bgate", l), i * 8 + c))
                            pp_, ppb_ = psum("b")
                            for k in range(kcs[i]):
                                mm(pp_, wbr[:, koffs[i] + k, cc * 128:(cc + 1) * 128], yT[:, koffs[i] + k, sl], k == 0, k == kcs[i] - 1,
                                   [wbr_b[i], yT_b[koffs[i] + k][tt]], [ppb_])
                            if i == 0:
                                tt_(ac, pp_, g_, ALU.mult, [ppb_, g_b], [acb])
                            else:
                                tm, tmb = tmpm[tmi % 2], tmpm_b[tmi % 2]; tmi += 1
                                tt_(tm, pp_, g_, ALU.mult, [ppb_, g_b], [tmb])
                                if i < 3:
                                    tt_(ac, ac, tm, ALU.add, [acb, tmb], [acb])
                                else:
                                    tt_(mg[:, cc, sl], ac, tm, ALU.add, [acb, tmb], [mg_b[cc][tt]])
                for tt in range(NT):
                    sl = slice(tt * TT, (tt + 1) * TT)
                    for c2 in range(KC):
                        po_, pob_ = psum("c")
                        for cc in range(2):
                            mm(po_, wo[:, cc, c2 * 128:(c2 + 1) * 128], mg[:, cc, sl], cc == 0, cc == 1, [wo_b, mg_b[cc][tt]], [pob_])
                        tt_(xT[:, c2, sl], xT[:, c2, sl], po_, ALU.add, [xT_b[c2][tt], pob_], [xT_b[c2][tt]])
            A.release()
            A.release()
            if dbg and sq_i == 0:
                debug_dump(f"xmix_{l}", xT, [128, KC, S], F32, flat(xT_b))
            if stop_after == ("mix", l):
                break

            P.barrier(); A.mark()
            if l == 1 and ROUTED:
                I32 = mybir.dt.int32
                IDX = A.alloc((NCH, 2), F32).bitcast(I32); idx_b = bufs("idx", NCH)
                W12 = A.alloc((NCH, 2), F32); w12_b = bufs("w12", NCH)
                cnt_i = A.alloc((8,), F32).bitcast(I32); cnt_b = Buf("cnt")
                A.mark()
                hT = A.alloc((KC, S), BF16); hT_b = bufs("hT2", KC, NT)
                sqtmp = [A.alloc((TT,), F32) for _ in range(2)]; sq_b = bufs("sq", 2)
                rmsnorm_to_h(("nffn", l), hT, hT_b, sqtmp, sq_b)
                wr = A.alloc((KC, NEXP), F32); wr_b = Buf("wr")
                rt = A.alloc((96,), F32); rt_b = Buf("rt")
                selb = A.alloc((8,), BF16); selb_b = Buf("selb")
                selacc = A.alloc((8,), BF16); selacc_b = Buf("selacc")
                htok = [A.alloc((D,), BF16) for _ in range(2)]; htok_b = bufs("htok", 2)
                dma("sp", wr, router_d[0].rearrange("(c p) e -> p c e", p=128), (), [wr_b], "wr")
                for c in range(KC):
                    ts_(wr[:, c, :], wr[:, c, :], vcol(("nffn", l), c), None, ALU.mult, None, [wr_b, vecs_b], [wr_b])
                memset(selacc, 0.0, [selacc_b])
                for ch in range(NCH):
                    tt = ch // 4
                    csl = slice(ch * 128, (ch + 1) * 128)
                    pl_, plb_ = psum("a")
                    for c in range(KC):
                        mm(pl_[:, 0:NEXP], xT[:, c, csl], wr[:, c, :], c == 0, c == KC - 1, [xT_b[c][tt], wr_b], [plb_])
                    mm(pl_[:, 16:17], rstd[:, csl], c256[:, 0:1], True, True, [rstd_b[tt], cf_b], [plb_])
                    ts_(rt[:, 16:17], pl_[:, 16:17], 2.0, None, ALU.mult, None, [plb_], [rt_b])
                    ts_(rt[:, 0:8], pl_[:, 0:8], rt[:, 16:17], None, ALU.mult, None, [plb_, rt_b], [rt_b])
                    P.emit("dve", lambda e, o=rt[:, 8:16], i=rt[:, 0:8]: e.max(out=o, in_=i), [rt_b], [rt_b])
                    ts_(rt[:, 17:18], rt[:, 8:9], -1.0, None, ALU.mult, None, [rt_b], [rt_b])
                    ts_(rt[:, 24:32], rt[:, 0:8], rt[:, 9:10], None, ALU.is_ge, None, [rt_b], [rt_b])
                    act(rt[:, 32:40], rt[:, 0:8], AF.Exp, [rt_b], [rt_b], bias=rt[:, 17:18])
                    tt_(rt[:, 32:40], rt[:, 32:40], rt[:, 24:32], ALU.mult, [rt_b], [rt_b])
                    P.emit("dve", lambda e, o=rt[:, 40:41], i=rt[:, 32:40]: e.tensor_reduce(out=o, in_=i, axis=AX.X, op=ALU.add), [rt_b], [rt_b])
                    P.emit("dve", lambda e, o=rt[:, 41:42], i=rt[:, 40:41]: e.reciprocal(out=o, in_=i), [rt_b], [rt_b])
                    ts_(rt[:, 48:56], rt[:, 32:40], rt[:, 41:42], None, ALU.mult, None, [rt_b], [rt_b])
                    dcopy(selb, rt[:, 24:32], [rt_b], [selb_b])
                    pr_, prb_ = psum("b")
                    mm(pr_[:, 0:8], ustrict, selb, True, False, [cb_b, selb_b], [prb_])
                    mm(pr_[:, 0:8], ones_b16, selacc, False, True, [cb_b, selacc_b], [prb_])
                    tt_(rt[:, 56:64], pr_[:, 0:8], offs, ALU.add, [prb_, cf_b], [rt_b])
                    tt_(rt[:, 56:64], rt[:, 56:64], rt[:, 24:32], ALU.mult, [rt_b], [rt_b])
                    tt_(selacc, selacc, selb, ALU.add, [selacc_b, selb_b], [selacc_b])
                    P.emit("dve", lambda e, o=rt[:, 64:72], i=rt[:, 56:64]: e.max(out=o, in_=i), [rt_b], [rt_b])
                    ts_(IDX[:, ch, :], rt[:, 64:66], -1.0, None, ALU.add, None, [rt_b], [idx_b[ch]])
                    for kk in range(2):
                        ts_(rt[:, 72:80], rt[:, 56:64], rt[:, 64 + kk:65 + kk], None, ALU.is_equal, None, [rt_b], [rt_b])
                        tt_(rt[:, 72:80], rt[:, 72:80], rt[:, 48:56], ALU.mult, [rt_b], [rt_b])
                        P.emit("dve", lambda e, o=W12[:, ch, kk:kk + 1], i=rt[:, 72:80]: e.tensor_reduce(out=o, in_=i, axis=AX.X, op=ALU.add),
                               [rt_b], [w12_b[ch]])
                    pt_, ptb_ = psum("c")
                    ptv = pt_.bitcast(BF16)
                    for c in range(KC):
                        P.emit("pe", lambda e, o=ptv[:, c * 128:(c + 1) * 128], i=hT[:, c, csl]: e.transpose(o, i, ident_b),
                               [hT_b[c][tt], identb_b], [ptb_])
                    hk, hkb = htok[ch % 2], htok_b[ch % 2]
                    dcopy(hk, ptv[:, 0:D], [ptb_], [hkb])
                    for kk in range(2):
                        P.emit("pool", lambda e, o=hs_d[:, :], ix=IDX[:, ch, kk:kk + 1], i=hk: e.indirect_dma_start(
                            out=o, out_offset=bass.IndirectOffsetOnAxis(ap=ix, axis=0), in_=i, in_offset=None),
                            [hkb, idx_b[ch]], [hs_bs[ch]], dma_key="hs_sc")
                pc_, pcb_ = psum("b")
                mm(pc_[:, 0:8], ones_b16, selacc, True, True, [cb_b, selacc_b], [pcb_])
                dcopy(cnt_i, pc_[:, 0:8], [pcb_], [cnt_b])
                A.release()
                if dbg and sq_i == 0:
                    debug_dump("cnt", cnt_i, [128, 8], I32, [cnt_b])
                    debug_dump("idx", IDX, [128, NCH, 2], I32, flat(idx_b))
                    debug_dump("w12", W12, [128, NCH, 2], F32, flat(w12_b))

                P.barrier(); A.mark()
                acc = A.alloc((KC, 1024), F32); acc_b = bufs("acc", 3)
                hTe = A.alloc((KC, 1024), BF16); hTe_b = bufs("hTe", 3)
                w1s = [A.alloc((KC, 512), BF16) for _ in range(2)]; w1_b = bufs("w1s", 2)
                w3s = [A.alloc((KC, 512), BF16) for _ in range(2)]; w3_b = bufs("w3s", 2)
                w2s = [A.alloc((4, D), BF16) for _ in range(2)]; w2_b = bufs("w2s", 2)
                aT = [A.alloc((4, TT), BF16) for _ in range(2)]; aT_b = bufs("aT", 2, 4)
                sil = [A.alloc((TT,), F32) for _ in range(2)]; sil_b = bufs("sil", 2)
                NHS = 4
                hsin = [A.alloc((D,), BF16) for _ in range(NHS)]; hsin_b = bufs("hsin", NHS)
                ytoks = [A.alloc((D,), F32) for _ in range(2)]; ytoks_b = bufs("ytok", 2)
                yti = 0
                NFB = DFE // 512
                wsl = 0
                hsi = 0
                ai = [0]
                for st, ex in [(st_, ex_) for st_ in range(2) for ex_ in range(NEXP)]:
                    for eng_ in ENGS:
                        P.emit(eng_, lambda e, en=eng_, a=cnt_i[0:1, ex:ex + 1]: e.reg_load(P.REG[en], a), [cnt_b], ())
                    W1, W3, W2 = mw1_d[0, ex], mw3_d[0, ex], mw2_d[0, ex]
                    if st == 1:
                        P.cond_begin(1024)
                    if st == 0:
                        pieces = [(0, 0, 512, None), (1, 512, 256, 512), (1, 768, 256, 768)]
                    else:
                        pieces = [(2, 0, 512, None), (3, 512, 512, None)]

                    def grp(thr):
                        if thr is not None:
                            P.cond_begin(thr)

                    def endgrp(thr):
                        if thr is not None:
                            P.cond_end()

                    for pi, (k, pc0, pw, thr) in enumerate(pieces):
                        grp(thr)
                        for sb in range(pw // 128):
                            hi_, hib_ = hsin[hsi % NHS], hsin_b[hsi % NHS]; hsi += 1
                            c0_ = pc0 + sb * 128
                            r0_ = ex * CAP + st * 1024 + c0_
                            dma("sp", hi_, hs_d[r0_:r0_ + 128, :], hs_bs, [hib_], f"hsin{(hsi - 1) % NHS}")
                            pt_, ptb_ = psum("b")
                            ptv = pt_.bitcast(BF16)
                            for c in range(KC):
                                P.emit("pe", lambda e, o=ptv[:, c * 128:(c + 1) * 128], i=hi_[:, c * 128:(c + 1) * 128]: e.transpose(o, i, ident_b),
                                       [hib_, identb_b], [ptb_])
                            dcopy(hTe[:, :, c0_:c0_ + 128], ptv[:, 0:D].rearrange("p (c t) -> p c t", c=KC), [ptb_], [hTe_b[pi]])
                        endgrp(thr)
                    units = [(fb, pi) for fb in range(NFB) for pi in range(len(pieces))]
                    ust = {}
                    wbase = wsl
                    wsl += NFB

                    def up(fb, pi):
                        s_ = (wbase + fb) % 2
                        if pi == 0:
                            f0 = fb * 512
                            wload(w1s[s_], w1_b[s_], W1, f0, 512, KC, f"w1s{s_}")
                            wload(w3s[s_], w3_b[s_], W3, f0, 512, KC, f"w3s{s_}")
                            src = W2.rearrange("(c p) n -> p c n", p=128)[:, f0 // 128:f0 // 128 + 4, :]
                            dma("pool", w2s[s_], src, (), [w2_b[s_]], f"w2s{s_}")
                        k, pc0, pw, thr = pieces[pi]
                        grp(thr)
                        a_, a_b = aT[ai[0] % 2], aT_b[ai[0] % 2]; ai[0] += 1
                        ust[(fb, pi)] = (a_, a_b)
                        for j in range(4):
                            p1, p1b = psum("a")
                            for c in range(KC):
                                mm(p1[:, 0:pw], w1s[s_][:, c, j * 128:(j + 1) * 128], hTe[:, c, pc0:pc0 + pw], c == 0, c == KC - 1,
                                   [w1_b[s_], hTe_b[pi]], [p1b])
                            p3, p3b = psum("d")
                            for c in range(KC):
                                mm(p3[:, 0:pw], w3s[s_][:, c, j * 128:(j + 1) * 128], hTe[:, c, pc0:pc0 + pw], c == 0, c == KC - 1,
                                   [w3_b[s_], hTe_b[pi]], [p3b])
                            sl_, sl_b = sil[j % 2], sil_b[j % 2]
                            act(sl_[:, 0:pw], p1[:, 0:pw], AF.Silu, [p1b], [sl_b])
                            tt_(a_[:, j, 0:pw], sl_[:, 0:pw], p3[:, 0:pw], ALU.mult, [sl_b, p3b], [a_b[j]])
                        endgrp(thr)

                    def down(fb, pi):
                        s_ = (wbase + fb) % 2
                        k, pc0, pw, thr = pieces[pi]
                        a_, a_b = ust.pop((fb, pi))
                        grp(thr)
                        for c2 in range(KC):
                            po_, pob_ = psum("c")
                            for j in range(4):
                                mm(po_[:, 0:pw], w2s[s_][:, j, c2 * 128:(c2 + 1) * 128], a_[:, j, 0:pw], j == 0, j == 3, [w2_b[s_], a_b[j]], [pob_])
                            av = acc[:, c2, pc0:pc0 + pw]
                            if fb == 0:
                                dcopy(av, po_[:, 0:pw], [pob_], [acc_b[pi]])
                            else:
                                tt_(av, av, po_[:, 0:pw], ALU.add, [acc_b[pi], pob_], [acc_b[pi]])
                        endgrp(thr)

                    up(*units[0])
                    for ui in range(len(units)):
                        if ui + 1 < len(units):
                            up(*units[ui + 1])
                        down(*units[ui])
                    for pi, (k, pc0, pw, thr) in enumerate(pieces):
                        grp(thr)
                        for sb in range(pw // 128):
                            c0_ = pc0 + sb * 128
                            ytok, ytok_b = ytoks[yti % 2], ytoks_b[yti % 2]; yti += 1
                            for hf in range(2):
                                pt_, ptb_ = psum("b")
                                for c4 in range(4):
                                    c = hf * 4 + c4
                                    P.emit("pe", lambda e, o=pt_[:, c4 * 128:(c4 + 1) * 128], i=acc[:, c, c0_:c0_ + 128]: e.transpose(o, i, ident),
                                           [acc_b[pi], cf_b], [ptb_])
                                dcopy(ytok[:, hf * 512:(hf + 1) * 512], pt_, [ptb_], [ytok_b])
                            r0_ = ex * CAP + st * 1024 + c0_
                            dma("sp", ys_d[r0_:r0_ + 128, :], ytok, [ytok_b], [ys_bs[ex * 4 + k]], f"ys_wr{(yti - 1) % 2}")
                        endgrp(thr)
                    if st == 1:
                        P.cond_end()
                A.release()

                P.barrier(); A.mark()
                yg = [[A.alloc((D,), F32) for _ in range(2)] for _ in range(2)]; yg_b = bufs("yg", 2, 2)
                fo = [A.alloc((D,), F32) for _ in range(2)]; fo_b = bufs("fo", 2)
                for ch in range(NCH):
                    tt = ch // 4
                    csl = slice(ch * 128, (ch + 1) * 128)
                    r_ = ch % 2
                    for kk in range(2):
                        P.emit("pool", lambda e, o=yg[r_][kk], ix=IDX[:, ch, kk:kk + 1], i=ys_d[:, :]: e.indirect_dma_start(
                            out=o, out_offset=None, in_=i, in_offset=bass.IndirectOffsetOnAxis(ap=ix, axis=0)),
                            ys_bs + [idx_b[ch]], [yg_b[r_][kk]], dma_key=f"yg{r_}{kk}")
                    f_, f_b = fo[r_], fo_b[r_]
                    ts_(f_, yg[r_][0], W12[:, ch, 0:1], None, ALU.mult, None, [yg_b[r_][0], w12_b[ch]], [f_b])
                    stt_(f_, yg[r_][1], W12[:, ch, 1:2], f_, ALU.mult, ALU.add, [yg_b[r_][1], w12_b[ch], f_b], [f_b])
                    for hf in range(2):
                        pt_, ptb_ = psum("b")
                        for c4 in range(4):
                            c = hf * 4 + c4
                            P.emit("pe", lambda e, o=pt_[:, c4 * 128:(c4 + 1) * 128], i=f_[:, c * 128:(c + 1) * 128]: e.transpose(o, i, ident),
                                   [f_b, cf_b], [ptb_])
                        for c4 in range(4):
                            c = hf * 4 + c4
                            tt_(xT[:, c, csl], xT[:, c, csl], pt_[:, c4 * 128:(c4 + 1) * 128], ALU.add, [xT_b[c][tt], ptb_], [xT_b[c][tt]])
                A.release()
                A.release()
                if dbg and sq_i == 0:
                    debug_dump(f"xffn_{l}", xT, [128, KC, S], F32, flat(xT_b))
                continue
            hT = A.alloc((KC, S), BF16); hT_b = bufs("hT2", KC, NT)
            sqtmp = [A.alloc((TT,), F32) for _ in range(2)]; sq_b = bufs("sq", 2)
            rmsnorm_to_h(("nffn", l), hT, hT_b, sqtmp, sq_b)
            w1s = [A.alloc((KC, 512), BF16) for _ in range(2)]; w1_b = bufs("w1s", 2)
            w3s = [A.alloc((KC, 512), BF16) for _ in range(2)]; w3_b = bufs("w3s", 2)
            w2s = [A.alloc((4, D), BF16) for _ in range(2)]; w2_b = bufs("w2s", 2)
            aT = [A.alloc((4, TT), BF16) for _ in range(2)]; aT_b = bufs("aT", 2, 4)
            sil = [A.alloc((TT,), F32) for _ in range(2)]; sil_b = bufs("sil", 2)
            moe = (l == 1)
            if moe:
                cwb = A.alloc((S,), F32); cwb_b = bufs("cwb", NT)
                cwT = A.alloc((S,), F32); cwT_b = bufs("cwT", NT)
                wr = A.alloc((KC, NEXP), F32); wr_b = Buf("wr")
                rt = A.alloc((64,), F32); rt_b = Buf("rt")
                dma("sp", wr, router_d[0].rearrange("(c p) e -> p c e", p=128), (), [wr_b], "wr")
                for c in range(KC):
                    ts_(wr[:, c, :], wr[:, c, :], vcol(("nffn", l), c), None, ALU.mult, None, [wr_b, vecs_b], [wr_b])
                for ch in range(NCH):
                    tt = ch // 4
                    csl = slice(ch * 128, (ch + 1) * 128)
                    pl_, plb_ = psum("a")
                    for c in range(KC):
                        mm(pl_[:, 0:NEXP], xT[:, c, csl], wr[:, c, :], c == 0, c == KC - 1, [xT_b[c][tt], wr_b], [plb_])
                    mm(pl_[:, 16:17], rstd[:, csl], c256[:, 0:1], True, True, [rstd_b[tt], cf_b], [plb_])
                    ts_(rt[:, 16:17], pl_[:, 16:17], 2.0, None, ALU.mult, None, [plb_], [rt_b])
                    ts_(rt[:, 0:8], pl_[:, 0:8], rt[:, 16:17], None, ALU.mult, None, [plb_, rt_b], [rt_b])
                    P.emit("dve", lambda e, o=rt[:, 8:16], i=rt[:, 0:8]: e.max(out=o, in_=i), [rt_b], [rt_b])
                    ts_(rt[:, 17:18], rt[:, 8:9], -1.0, None, ALU.mult, None, [rt_b], [rt_b])
                    ts_(rt[:, 24:32], rt[:, 0:8], rt[:, 9:10], None, ALU.is_ge, None, [rt_b], [rt_b])
                    act(rt[:, 32:40], rt[:, 0:8], AF.Exp, [rt_b], [rt_b], bias=rt[:, 17:18])
                    tt_(rt[:, 32:40], rt[:, 32:40], rt[:, 24:32], ALU.mult, [rt_b], [rt_b])
                    P.emit("dve", lambda e, o=rt[:, 40:41], i=rt[:, 32:40]: e.tensor_reduce(out=o, in_=i, axis=AX.X, op=ALU.add), [rt_b], [rt_b])
                    P.emit("dve", lambda e, o=rt[:, 41:42], i=rt[:, 40:41]: e.reciprocal(out=o, in_=i), [rt_b], [rt_b])
                    ts_(rt[:, 48:56], rt[:, 32:40], rt[:, 41:42], None, ALU.mult, None, [rt_b], [rt_b])
                    ptr, ptrb = psum("b")
                    P.emit("pe", lambda e, o=ptr[0:8, 0:128], i=rt[:, 48:56]: e.transpose(o, i, ident), [rt_b, cf_b], [ptrb])
                    dcopy(cwT[0:8, csl], ptr[0:8, 0:128], [ptrb], [cwT_b[tt]])
                experts = [(mw1_d[0, e], mw3_d[0, e], mw2_d[0, e], DFE, e) for e in range(NEXP)]
            else:
                experts = [(dw1_d[0], dw3_d[0], dw2_d[0], DFF, None)]
            steps = []
            for (W1, W3, W2, dff, e) in experts:
                f0 = 0
                while f0 < dff:
                    n = min(512, dff - f0)
                    steps.append((W1, W3, W2, f0, n, e))
                    f0 += n

            def load_step(si):
                W1, W3, W2, f0, n, e = steps[si]
                s_ = si % 2
                wload(w1s[s_], w1_b[s_], W1, f0, n, KC, f"w1s{s_}")
                wload(w3s[s_], w3_b[s_], W3, f0, n, KC, f"w3s{s_}")
                src = W2.rearrange("(c p) n -> p c n", p=128)[:, f0 // 128:f0 // 128 + n // 128, :]
                dma("pool", w2s[s_][:, 0:n // 128, :], src, (), [w2_b[s_]], f"w2s{s_}")

            load_step(0)
            ai = 0
            cur_e = -1
            for si, (W1, W3, W2, f0, n, e) in enumerate(steps):
                if si + 1 < len(steps):
                    load_step(si + 1)
                s_ = si % 2
                nj = n // 128
                if moe and e != cur_e:
                    cur_e = e
                    for tt in range(NT):
                        sl = slice(tt * TT, (tt + 1) * TT)
                        pc_, pcb_ = psum("b")
                        mm(pc_, sel[0:8, e * 128:(e + 1) * 128], cwT[0:8, sl], True, True, [cf_b, cwT_b[tt]], [pcb_])
                        dcopy(cwb[:, sl], pc_, [pcb_], [cwb_b[tt]])
                for tt in range(NT):
                    sl = slice(tt * TT, (tt + 1) * TT)
                    a_, a_b = aT[ai % 2], aT_b[ai % 2]; ai += 1
                    for j in range(nj):
                        p1, p1b = psum("a")
                        proj_fm(p1, p1b, w1s[s_], w1_b[s_], j * 128, hT, hT_b, tt)
                        p3, p3b = psum("d")
                        proj_fm(p3, p3b, w3s[s_], w3_b[s_], j * 128, hT, hT_b, tt)
                        sl_, sl_b = sil[j % 2], sil_b[j % 2]
                        act(sl_, p1, AF.Silu, [p1b], [sl_b])
                        if moe:
                            tt_(sl_, sl_, cwb[:, sl], ALU.mult, [sl_b, cwb_b[tt]], [sl_b])
                        tt_(a_[:, j, :], sl_, p3, ALU.mult, [sl_b, p3b], [a_b[j]])
                    for c2 in range(KC):
                        po_, pob_ = psum("c")
                        for j in range(nj):
                            mm(po_, w2s[s_][:, j, c2 * 128:(c2 + 1) * 128], a_[:, j, :], j == 0, j == nj - 1, [w2_b[s_], a_b[j]], [pob_])
                        tt_(xT[:, c2, sl], xT[:, c2, sl], po_, ALU.add, [xT_b[c2][tt], pob_], [xT_b[c2][tt]])
            A.release()
            if dbg and sq_i == 0:
                debug_dump(f"xffn_{l}", xT, [128, KC, S], F32, flat(xT_b))
        else:
            P.barrier(); A.mark()
            sqtmp = [A.alloc((TT,), F32) for _ in range(2)]; sq_b = bufs("sq", 2)
            rmsnorm_to_h(None, None, None, sqtmp, sq_b)
            for tt in range(NT):
                sl = slice(tt * TT, (tt + 1) * TT)
                for c in range(KC):
                    stt_(xT[:, c, sl], xT[:, c, sl], vcol("nfinal", c), rstd[:, sl], ALU.mult, ALU.mult,
                         [xT_b[c][tt], rstd_b[tt], vecs_b], [xT_b[c][tt]])
            for c in range(KC):
                dma("sp", out_d[sq_i, c], xT[:, c, :], xT_b[c], (), f"out{c}")
            A.release()
            P.barrier()
            continue
        break

    P.finalize(nc, stack)
    stack.close()
    build_program.peak = A.peak
    return nc, list(dbg_out.keys())


def _fm(v):
    return np.ascontiguousarray(v.reshape(-1, 128).T)


def host_consts_and_layout(inp):
    f32 = np.float32
    vecs = np.zeros((128, NV), f32)
    for l in range(2):
        vecs[:, VL[("nmix", l)]:VL[("nmix", l)] + 8] = _fm(inp["norm_mix"][l])
        vecs[:, VL[("nffn", l)]:VL[("nffn", l)] + 8] = _fm(inp["norm_ffn"][l])
        vecs[:, VL[("bgate", l)]:VL[("bgate", l)] + 32] = _fm(inp["b_gate"][l])
        vecs[:, VL[("poolb", l)]:VL[("poolb", l)] + 2] = _fm(inp["pool_b"][l].reshape(-1))
        vecs[:, VL[("pools", l)]:VL[("pools", l)] + 2] = _fm(inp["pool_scale"][l])
        cw = inp["conv_w"][l]
        for j in range(2):
            vecs[:, VL[("convw", l)] + j * 31:VL[("convw", l)] + (j + 1) * 31] = cw[:, j * 128:(j + 1) * 128].T
        vecs[:, VL[("convb", l)]:VL[("convb", l)] + 2] = _fm(inp["conv_b"][l])
        vecs[:, VL[("clng", l)]:VL[("clng", l)] + 2] = _fm(inp["conv_ln_g"][l])
        vecs[:, VL[("clnb", l)]:VL[("clnb", l)] + 2] = _fm(inp["conv_ln_b"][l])
    vecs[:, VL["nfinal"]:VL["nfinal"] + 8] = _fm(inp["norm_final"])
    wins = np.array([[2, 4], [8, 16]], f32)
    for j in range(2):
        w = np.where(np.arange(128) < 64, wins[j, 0], wins[j, 1]).astype(f32)
        vecs[:, VL["pinvw"] + j] = 1.0 / w
        t = np.arange(16, dtype=f32)[None, :]
        vecs[:, VL["pinv16"] + j * 16:VL["pinv16"] + (j + 1) * 16] = 1.0 / np.minimum(t + 1.0, w[:, None])
    rows = np.zeros((128, 2 * NROW), f32)
    for l in range(2):
        r0 = l * NROW
        rows[:, r0:r0 + 256] = inp["gmlp_ln_g"][l][None, :]
        rows[:, r0 + 256:r0 + 512] = inp["gmlp_ln_b"][l][None, :]
        rows[:, r0 + 512:r0 + 1024] = inp["gmlp_b_s"][l].reshape(1, 512)
    cf = np.zeros((128, CFW), f32)
    cf[:, 0:128] = np.eye(128, dtype=f32)
    cf[:, 128:256] = 1.0
    cf[:, 256:384] = 1.0 / 256.0
    sidx = np.arange(128)
    cf[:, 384:512] = (sidx[:, None] <= sidx[None, :]).astype(f32)
    for e in range(8):
        cf[e, 512 + e * 128:512 + (e + 1) * 128] = 1.0
        cf[:, 1536 + e] = e * CAP + 1.0
    cb = np.zeros((128, 256), f32)
    cb[:, 0:128] = (sidx[:, None] < sidx[None, :]).astype(f32)
    cb[:, 128:256] = 1.0
    cb = cb.astype(ml_dtypes.bfloat16)
    slopes = 2.0 ** (-8.0 * np.arange(1, 9) / 8.0)
    o = np.arange(16)[None, :, None]; tl = np.arange(128)[None, None, :]; sl = np.arange(128)[:, None, None]
    delta = 128 * o + tl - sl
    mult = ((delta <= 128).astype(np.float64) + ((delta % 4 == 0) & (delta <= 512)) + ((delta % 16 == 0) & (delta <= 2048)))
    mult = np.where(delta >= 0, mult, 0.0)
    mtab = np.stack([mult * np.exp(-s * np.maximum(delta, 0)) for s in slopes]).reshape(8, 128, 2048)
    mtab = mtab.astype(ml_dtypes.bfloat16)
    wst = np.ascontiguousarray(np.transpose(inp["gmlp_w_s"], (0, 3, 1, 2)))
    common = {
        "vecs": vecs, "rows": rows, "cf32": cf, "cb16": cb, "mtab": mtab, "wst": wst,
        "w_in": inp["w_in"], "pool_w": inp["pool_w"],
        "w_br_a": inp["w_br_a"], "w_br_b": inp["w_br_b"], "w_br_c": inp["w_br_c"], "w_br_d": inp["w_br_d"],
        "w_gate": inp["w_gate"], "w_out": inp["w_out"],
        "dense_w1": inp["dense_w1"], "dense_w3": inp["dense_w3"], "dense_w2": inp["dense_w2"],
        "moe_router": inp["moe_router"], "moe_w1": inp["moe_w1"], "moe_w3": inp["moe_w3"], "moe_w2": inp["moe_w2"],
    }
    return common


def x_to_fm(xs):
    n = xs.shape[0]
    return np.ascontiguousarray(xs.reshape(n, S, KC, 128).transpose(0, 2, 3, 1))


def fm_to_x(o):
    n = o.shape[0]
    return np.ascontiguousarray(o.transpose(0, 3, 1, 2).reshape(n, S, D))


_CACHE = {}


def kernel(**inputs):
    inp = {k: np.asarray(v) for k, v in inputs.items()}
    x = inp["x"].astype(np.float32, copy=False)
    B = x.shape[0]
    nseq = B // NCORES
    common = host_consts_and_layout(inp)
    if "nc" not in _CACHE:
        _CACHE["nc"] = build_program(nseq)[0]
    nc = _CACHE["nc"]
    in_maps = []
    for c in range(NCORES):
        m = dict(common)
        m["xT"] = x_to_fm(x[c * nseq:(c + 1) * nseq])
        in_maps.append(m)
    res = run_bass_kernel_spmd(nc, in_maps, core_ids=list(range(NCORES)))
    outs = [fm_to_x(np.asarray(r["outT"])) for r in res.results]
    return np.concatenate(outs, axis=0).astype(np.float32)
```
